# Optimizing a Trainium2 kernel written in Bass

```python
import math
import jax, jax.numpy as jnp
from jax import lax
import numpy as np

D_MODEL = 2048
BATCH = 4
SEQ = 4096
DEPTH = 2

GRID_W = 64
CTX_LEN = 256
EPS = 1e-6
CHUNK = 64
HY_W = 512
HY_ORDER = 2
HY_SHORT = 3
HY_EMB = 33
HY_FFN = 64
HY_TARGET = 1e-2
HY_FAST_PCT = 0.3
HY_SLOW_PCT = 1.5
GLA_HEADS = 4
GLA_DK = 64
GLA_DV = 128
GLA_RANK = 16
GLA_TAU = 16.0
ML_HEADS = 4
ML_D = 128
RET_HEADS = 4
RET_D = 128
ROPE_BASE = 10000.0
MOE_GROUPS = 4
MOE_EPG = 8
MOE_TOPK = 2
MOE_FF = 256

D_MIX = HY_W + GLA_HEADS * GLA_DV + ML_HEADS * ML_D + RET_HEADS * RET_D
PROJ_SPLITS = (
    ('hy', (HY_ORDER + 1) * HY_W),
    ('gla_q', GLA_HEADS * GLA_DK), ('gla_k', GLA_HEADS * GLA_DK),
    ('gla_v', GLA_HEADS * GLA_DV), ('gla_g', GLA_HEADS * GLA_DV), ('gla_r', 2 * GLA_RANK),
    ('ml_q', ML_HEADS * ML_D), ('ml_k', ML_HEADS * ML_D), ('ml_v', ML_HEADS * ML_D),
    ('ml_o', ML_HEADS * ML_D), ('ml_gates', 2 * 2 * ML_HEADS),
    ('ret_q', RET_HEADS * RET_D), ('ret_k', RET_HEADS * RET_D),
    ('ret_v', RET_HEADS * RET_D), ('ret_g', RET_HEADS * RET_D),
)
D_PROJ = sum(w for _, w in PROJ_SPLITS)

kernel_name = 'hybrid_bidir_hyena_gla_mlstm_retnet_hmoe'

F32 = jnp.float32


def rmsnorm(x, g):
    x = x.astype(F32)
    return x * lax.rsqrt(jnp.mean(x * x, axis=-1, keepdims=True) + EPS) * g


def head_rmsnorm(y, g, n_heads):
    b, l, w = y.shape
    yh = y.reshape(b, l, n_heads, w // n_heads)
    yh = yh * lax.rsqrt(jnp.mean(yh * yh, axis=-1, keepdims=True) + EPS)
    return yh.reshape(b, l, w) * g


def split_proj(p):
    out, off = {}, 0
    for name, width in PROJ_SPLITS:
        out[name] = p[..., off:off + width]
        off += width
    return out


def to_heads(a, n_heads):
    b, l, w = a.shape
    return a.reshape(b, l, n_heads, w // n_heads).transpose(0, 2, 1, 3)


def from_heads(a):
    b, h, l, d = a.shape
    return a.transpose(0, 2, 1, 3).reshape(b, l, h * d)


def short_conv(u, w, b):
    k = w.shape[0]
    y = lax.conv_general_dilated(u, w[:, None, :].astype(u.dtype), window_strides=(1,),
                                 padding=((k // 2, k // 2),), dimension_numbers=('NWC', 'WIO', 'NWC'),
                                 feature_group_count=u.shape[-1])
    return y + b


def hyena_filters(length, lp):
    pos = jnp.arange(length, dtype=F32)
    t = pos / (length - 1)
    bands = (HY_EMB - 1) // 2
    fr = jnp.linspace(1e-4, bands - 1, bands, dtype=F32)
    ang = (2.0 * math.pi / length) * pos[:, None] * fr[None, :]
    z = jnp.concatenate([t[:, None], jnp.cos(ang), -jnp.sin(ang)], axis=-1)
    hdn = jnp.sin(lp['hy_f_freq'][0] * (z @ lp['hy_f_w1'] + lp['hy_f_b1']))
    hdn = jnp.sin(lp['hy_f_freq'][1] * (hdn @ lp['hy_f_w2'] + lp['hy_f_b2']))
    h = (hdn @ lp['hy_f_w3']) * jnp.exp(-t[:, None] * jnp.abs(lp['hy_decay']))
    return h.reshape(length, HY_ORDER, 2, HY_W)


def long_conv(z, h_fwd, h_bwd):
    length, ch = z.shape[1], z.shape[2]
    h_full = jnp.concatenate([h_fwd, jnp.zeros((1, ch), F32), jnp.flip(h_bwd[1:], axis=0)], axis=0)
    zf = jnp.fft.rfft(z, n=2 * length, axis=1)
    hf = jnp.fft.rfft(h_full, n=2 * length, axis=0)
    return jnp.fft.irfft(zf * hf[None], n=2 * length, axis=1)[:, :length]


def hyena_mixer(u, lp):
    length = u.shape[1]
    u = short_conv(u.astype(F32), lp['hy_conv_w'].astype(F32), lp['hy_conv_b'])
    parts = jnp.split(u, HY_ORDER + 1, axis=-1)
    filt = hyena_filters(length, lp)
    z = parts[0]
    for o in range(HY_ORDER):
        z = parts[o + 1] * (long_conv(z, filt[:, o, 0], filt[:, o, 1]) + lp['hy_skip'][o] * z)
    return z


def gla_chunked(q, k, v, log_a, s0):
    b_, h_, length, dk = q.shape
    dv = v.shape[-1]
    n = length // CHUNK
    rs = lambda a: a.astype(F32).reshape(b_, h_, n, CHUNK, a.shape[-1])
    q, k, v, log_a = rs(q), rs(k), rs(v), rs(log_a)
    bc = jnp.cumsum(log_a, axis=3)
    b_last = bc[:, :, :, -1:, :]
    q_in = q * jnp.exp(bc)
    k_in = k * jnp.exp(-bc)
    k_st = k * jnp.exp(b_last - bc)
    causal = jnp.tril(jnp.ones((CHUNK, CHUNK), dtype=bool))
    att = jnp.where(causal, jnp.einsum('bhnid,bhnjd->bhnij', q_in, k_in), 0.0)
    intra = jnp.einsum('bhnij,bhnje->bhnie', att, v)
    u = jnp.einsum('bhnjd,bhnje->bhnde', k_st, v)
    a_chunk = jnp.exp(b_last[:, :, :, 0, :])

    def step(s, xs):
        a_i, u_i = xs
        return a_i[..., None] * s + u_i, s

    s_fin, s_start = lax.scan(step, s0.astype(F32), (jnp.moveaxis(a_chunk, 2, 0), jnp.moveaxis(u, 2, 0)))
    s_start = jnp.moveaxis(s_start, 0, 2)
    inter = jnp.einsum('bhnid,bhnde->bhnie', q_in, s_start)
    return (intra + inter).reshape(b_, h_, length, dv), s_fin


def mlstm_chunked(q, k, v, ig, lf, state):
    b_, h_, length, d = q.shape
    n = length // CHUNK
    chunks = lambda a: jnp.moveaxis(a.astype(F32).reshape(b_, h_, n, CHUNK, *a.shape[3:]), 2, 0)
    causal = jnp.tril(jnp.ones((CHUNK, CHUNK), dtype=bool))

    def step(carry, xs):
        c_mat, n_vec, m = carry
        qc, kc, vc, ic, fc = xs
        bc = jnp.cumsum(fc, axis=-1)
        dmat = jnp.where(causal, bc[..., :, None] - bc[..., None, :] + ic[..., None, :], -jnp.inf)
        inter_log = bc + m[..., None]
        m_t = jnp.maximum(inter_log, jnp.max(dmat, axis=-1))
        s = jnp.einsum('bhid,bhjd->bhij', qc, kc) * jnp.exp(dmat - m_t[..., None])
        inter = jnp.exp(inter_log - m_t)
        num = inter[..., None] * jnp.einsum('bhid,bhde->bhie', qc, c_mat) + jnp.einsum('bhij,bhje->bhie', s, vc)
        den = jnp.abs(inter * jnp.einsum('bhid,bhd->bhi', qc, n_vec) + jnp.sum(s, axis=-1))
        h = num / jnp.maximum(den, jnp.exp(-m_t))[..., None]
        b_end = bc[..., -1]
        g_log = b_end[..., None] - bc + ic
        m_new = jnp.maximum(b_end + m, jnp.max(g_log, axis=-1))
        g = jnp.exp(g_log - m_new[..., None])
        dec = jnp.exp(b_end + m - m_new)
        c_new = dec[..., None, None] * c_mat + jnp.einsum('bhj,bhjd,bhje->bhde', g, kc, vc)
        n_new = dec[..., None] * n_vec + jnp.einsum('bhj,bhjd->bhd', g, kc)
        return (c_new, n_new, m_new), h

    state, h = lax.scan(step, state, tuple(chunks(a) for a in (q, k, v, ig, lf)))
    return jnp.moveaxis(h, 0, 2).reshape(b_, h_, length, d), state


def _flip(a):
    return jnp.flip(a, axis=2)


def run_bidirectional(scan_fn, ctx_qkv, ctx_gates, lat_qkv, lat_gates, init_state):
    out_ctx, out_lat = 0.0, 0.0
    for d in range(2):
        tr = _flip if d == 1 else (lambda a: a)
        o_c, st = scan_fn(*[tr(a) for a in ctx_qkv], *[tr(a) for a in ctx_gates[d]], init_state)
        o_l, _ = scan_fn(*[tr(a) for a in lat_qkv], *[tr(a) for a in lat_gates[d]], st)
        out_ctx = out_ctx + tr(o_c)
        out_lat = out_lat + tr(o_l)
    return out_ctx, out_lat


def gla_mixer(p_ctx, p_lat, lp, with_ctx):
    def prep(p):
        q = to_heads(p['gla_q'], GLA_HEADS) * GLA_DK ** -0.5
        k = to_heads(p['gla_k'], GLA_HEADS)
        v = to_heads(p['gla_v'], GLA_HEADS)
        gates = []
        for d in range(2):
            logit = p['gla_r'][..., d * GLA_RANK:(d + 1) * GLA_RANK] @ lp['gla_wa2'][d] + lp['gla_ba'][d]
            gates.append((to_heads(jax.nn.log_sigmoid(logit) / GLA_TAU, GLA_HEADS),))
        return (q, k, v), gates

    (cq, cg), (lq, lg) = prep(p_ctx), prep(p_lat)
    init = jnp.zeros((cq[0].shape[0], GLA_HEADS, GLA_DK, GLA_DV), F32)
    o_c, o_l = run_bidirectional(gla_chunked, cq, cg, lq, lg, init)
    post = lambda o, p: head_rmsnorm(from_heads(o), lp['gla_norm_g'], GLA_HEADS) * jax.nn.silu(p['gla_g'])
    return (post(o_c, p_ctx) if with_ctx else None), post(o_l, p_lat)


def mlstm_mixer(p_ctx, p_lat, lp, with_ctx):
    def prep(p):
        b_, l_ = p['ml_q'].shape[:2]
        q = to_heads(p['ml_q'], ML_HEADS)
        k = to_heads(p['ml_k'], ML_HEADS) * ML_D ** -0.5
        v = to_heads(p['ml_v'], ML_HEADS)
        g = p['ml_gates'].reshape(b_, l_, 2, 2, ML_HEADS) + lp['ml_gate_b']
        g = g.transpose(2, 3, 0, 4, 1)
        gates = [(g[d, 0], jax.nn.log_sigmoid(g[d, 1])) for d in range(2)]
        return (q, k, v), gates

    (cq, cg), (lq, lg) = prep(p_ctx), prep(p_lat)
    b_ = cq[0].shape[0]
    init = (jnp.zeros((b_, ML_HEADS, ML_D, ML_D), F32), jnp.zeros((b_, ML_HEADS, ML_D), F32),
            jnp.zeros((b_, ML_HEADS), F32))
    o_c, o_l = run_bidirectional(mlstm_chunked, cq, cg, lq, lg, init)
    post = lambda o, p: head_rmsnorm(jax.nn.sigmoid(p['ml_o']) * from_heads(o), lp['ml_norm_g'], ML_HEADS)
    return (post(o_c, p_ctx) if with_ctx else None), post(o_l, p_lat)


def rotary_2d(a):
    length, d = a.shape[2], a.shape[3]
    rows = length // GRID_W
    row = jnp.repeat(jnp.arange(rows, dtype=F32), GRID_W)
    col = jnp.tile(jnp.arange(GRID_W, dtype=F32), rows)
    nf = d // 4
    inv = ROPE_BASE ** (-jnp.arange(nf, dtype=F32) / nf)
    ang = jnp.concatenate([row[:, None] * inv, col[:, None] * inv], axis=-1)
    cos, sin = jnp.cos(ang), jnp.sin(ang)
    a1, a2 = a[..., :d // 2], a[..., d // 2:]
    return jnp.concatenate([a1 * cos - a2 * sin, a1 * sin + a2 * cos], axis=-1)


def retention_mixer(p_ctx, p_lat, lp, with_ctx):
    def prep(p, rotate):
        q = to_heads(p['ret_q'], RET_HEADS)
        k = to_heads(p['ret_k'], RET_HEADS)
        if rotate:
            q, k = rotary_2d(q), rotary_2d(k)
        q = q * RET_D ** -0.5
        v = to_heads(p['ret_v'], RET_HEADS)
        gates = [(jnp.broadcast_to(jax.nn.log_sigmoid(lp['ret_decay'][d])[None, :, None, None], q.shape),)
                 for d in range(2)]
        return (q, k, v), gates

    (cq, cg), (lq, lg) = prep(p_ctx, False), prep(p_lat, True)
    init = jnp.zeros((cq[0].shape[0], RET_HEADS, RET_D, RET_D), F32)
    o_c, o_l = run_bidirectional(gla_chunked, cq, cg, lq, lg, init)
    post = lambda o, p: head_rmsnorm(from_heads(o), lp['ret_norm_g'], RET_HEADS) * jax.nn.silu(p['ret_g'])
    return (post(o_c, p_ctx) if with_ctx else None), post(o_l, p_lat)


def token_mixers(p_ctx, p_lat, lp, with_ctx):
    lat_parts = [hyena_mixer(p_lat['hy'], lp)]
    ctx_parts = [hyena_mixer(p_ctx['hy'], lp)] if with_ctx else []
    for mixer in (gla_mixer, mlstm_mixer, retention_mixer):
        o_ctx, o_lat = mixer(p_ctx, p_lat, lp, with_ctx)
        lat_parts.append(o_lat)
        ctx_parts.append(o_ctx)
    y_lat = jnp.concatenate(lat_parts, axis=-1)
    y_ctx = jnp.concatenate(ctx_parts, axis=-1) if with_ctx else None
    return y_ctx, y_lat


def hier_moe(h, lp):
    b_, l_, d = h.shape
    t = h.reshape(-1, d).astype(F32)
    g_prob = jax.nn.softmax(t @ lp['moe_wg'] + lp['moe_bg'], axis=-1)
    pg, gidx = lax.top_k(g_prob, 1)
    e_logits = (t @ lp['moe_we'] + lp['moe_be']).reshape(-1, MOE_GROUPS, MOE_EPG)
    sel = jnp.take_along_axis(e_logits, gidx[:, :, None], axis=1)[:, 0]
    tv, ti = lax.top_k(sel, MOE_TOPK)
    w = jax.nn.softmax(tv, axis=-1) * pg
    esel = jnp.sum(jax.nn.one_hot(ti, MOE_EPG, dtype=F32) * w[..., None], axis=1)
    combine = jax.nn.one_hot(gidx[:, 0], MOE_GROUPS, dtype=F32)[:, :, None] * esel[:, None, :]
    y = jnp.zeros_like(t)
    for g in range(MOE_GROUPS):
        a = jnp.einsum('td,edf->tef', t, lp['moe_w1'][g])
        b = jnp.einsum('td,edf->tef', t, lp['moe_w3'][g])
        act = jax.nn.silu(a) * b * combine[:, g, :, None]
        y = y + jnp.einsum('tef,efd->td', act, lp['moe_w2'][g])
    return y.reshape(b_, l_, d)


def setup_inputs(seed: int = 0) -> dict:
    key = jax.random.key(seed)
    ks = iter(jax.random.split(key, 48))
    nrm = lambda shape, scale: jax.random.normal(next(ks), shape, F32) * scale
    gain = lambda shape: 1.0 + nrm(shape, 0.05)
    n_e = MOE_GROUPS * MOE_EPG
    dmin = -math.log(HY_TARGET) / HY_SLOW_PCT
    dmax = -math.log(HY_TARGET) / HY_FAST_PCT
    base_decay = jnp.tile(jnp.linspace(dmin, dmax, HY_W, dtype=F32), HY_ORDER * 2)
    ret_base = jnp.log(2.0 ** (5.0 + jnp.arange(RET_HEADS, dtype=F32)) - 1.0)
    f_base = jnp.linspace(3.0, 6.0, ML_HEADS, dtype=F32)
    return {
        'x': nrm((BATCH, SEQ, D_MODEL), 1.0),
        'c': nrm((BATCH, D_MODEL), 1.0),
        'ctx': nrm((BATCH, CTX_LEN, D_MODEL), 1.0),
        'c_ctx': nrm((D_MODEL,), 1.0),
        'ada_w': nrm((DEPTH, D_MODEL, 6 * D_MODEL), 0.5 * D_MODEL ** -0.5),
        'ada_b': nrm((DEPTH, 6 * D_MODEL), 0.02),
        'norm1_g': gain((DEPTH, D_MODEL)),
        'norm2_g': gain((DEPTH, D_MODEL)),
        'w_in': nrm((DEPTH, D_MODEL, D_PROJ), D_MODEL ** -0.5),
        'hy_conv_w': nrm((DEPTH, HY_SHORT, (HY_ORDER + 1) * HY_W), HY_SHORT ** -0.5),
        'hy_conv_b': nrm((DEPTH, (HY_ORDER + 1) * HY_W), 0.02),
        'hy_f_w1': nrm((DEPTH, HY_EMB, HY_FFN), HY_EMB ** -0.5),
        'hy_f_b1': nrm((DEPTH, HY_FFN), 0.1),
        'hy_f_w2': nrm((DEPTH, HY_FFN, HY_FFN), HY_FFN ** -0.5),
        'hy_f_b2': nrm((DEPTH, HY_FFN), 0.1),
        'hy_f_freq': 1.0 + nrm((DEPTH, 2, HY_FFN), 0.1),
        'hy_f_w3': nrm((DEPTH, HY_FFN, HY_ORDER * 2 * HY_W), 0.01),
        'hy_decay': base_decay * (1.0 + nrm((DEPTH, HY_ORDER * 2 * HY_W), 0.05)),
        'hy_skip': nrm((DEPTH, HY_ORDER, HY_W), 0.5),
        'gla_wa2': nrm((DEPTH, 2, GLA_RANK, GLA_HEADS * GLA_DK), GLA_RANK ** -0.5),
        'gla_ba': nrm((DEPTH, 2, GLA_HEADS * GLA_DK), 0.1),
        'gla_norm_g': gain((DEPTH, GLA_HEADS * GLA_DV)),
        'ml_gate_b': jnp.concatenate([nrm((DEPTH, 2, 1, ML_HEADS), 0.1),
                                      f_base + nrm((DEPTH, 2, 1, ML_HEADS), 0.1)], axis=2),
        'ml_norm_g': gain((DEPTH, ML_HEADS * ML_D)),
        'ret_decay': ret_base + nrm((DEPTH, 2, RET_HEADS), 0.05),
        'ret_norm_g': gain((DEPTH, RET_HEADS * RET_D)),
        'w_out': nrm((DEPTH, D_MIX, D_MODEL), D_MIX ** -0.5),
        'moe_wg': nrm((DEPTH, D_MODEL, MOE_GROUPS), D_MODEL ** -0.5),
        'moe_bg': nrm((DEPTH, MOE_GROUPS), 0.01),
        'moe_we': nrm((DEPTH, D_MODEL, n_e), D_MODEL ** -0.5),
        'moe_be': nrm((DEPTH, n_e), 0.01),
        'moe_w1': nrm((DEPTH, MOE_GROUPS, MOE_EPG, D_MODEL, MOE_FF), D_MODEL ** -0.5),
        'moe_w3': nrm((DEPTH, MOE_GROUPS, MOE_EPG, D_MODEL, MOE_FF), D_MODEL ** -0.5),
        'moe_w2': nrm((DEPTH, MOE_GROUPS, MOE_EPG, MOE_FF, D_MODEL), MOE_FF ** -0.5),
        'final_g': gain((D_MODEL,)),
    }


def reference(x, c, ctx, c_ctx, ada_w, ada_b, norm1_g, norm2_g, w_in, hy_conv_w, hy_conv_b,
              hy_f_w1, hy_f_b1, hy_f_w2, hy_f_b2, hy_f_freq, hy_f_w3, hy_decay, hy_skip,
              gla_wa2, gla_ba, gla_norm_g, ml_gate_b, ml_norm_g, ret_decay, ret_norm_g, w_out,
              moe_wg, moe_bg, moe_we, moe_be, moe_w1, moe_w3, moe_w2, final_g):
    lat = x.astype(F32)
    cx = ctx.astype(F32)
    for l in range(DEPTH):
        with_ctx = l < DEPTH - 1
        lp = {
            'hy_conv_w': hy_conv_w[l], 'hy_conv_b': hy_conv_b[l], 'hy_f_w1': hy_f_w1[l], 'hy_f_b1': hy_f_b1[l],
            'hy_f_w2': hy_f_w2[l], 'hy_f_b2': hy_f_b2[l], 'hy_f_freq': hy_f_freq[l], 'hy_f_w3': hy_f_w3[l],
            'hy_decay': hy_decay[l], 'hy_skip': hy_skip[l],
            'gla_wa2': gla_wa2[l], 'gla_ba': gla_ba[l], 'gla_norm_g': gla_norm_g[l],
            'ml_gate_b': ml_gate_b[l], 'ml_norm_g': ml_norm_g[l],
            'ret_decay': ret_decay[l], 'ret_norm_g': ret_norm_g[l],
            'moe_wg': moe_wg[l], 'moe_bg': moe_bg[l], 'moe_we': moe_we[l], 'moe_be': moe_be[l],
            'moe_w1': moe_w1[l], 'moe_w3': moe_w3[l], 'moe_w2': moe_w2[l],
        }
        mod = jax.nn.silu(c.astype(F32)) @ ada_w[l] + ada_b[l]
        mod_c = jax.nn.silu(c_ctx.astype(F32)) @ ada_w[l] + ada_b[l]
        sh1, sc1, g1, sh2, sc2, g2 = [m[:, None, :] for m in jnp.split(mod, 6, axis=-1)]
        csh1, csc1, cg1, csh2, csc2, cg2 = jnp.split(mod_c, 6, axis=-1)
        p_lat = split_proj((rmsnorm(lat, norm1_g[l]) * (1.0 + sc1) + sh1) @ w_in[l])
        p_ctx = split_proj((rmsnorm(cx, norm1_g[l]) * (1.0 + csc1) + csh1) @ w_in[l])
        y_ctx, y_lat = token_mixers(p_ctx, p_lat, lp, with_ctx)
        lat = lat + g1 * (y_lat @ w_out[l])
        lat = lat + g2 * hier_moe(rmsnorm(lat, norm2_g[l]) * (1.0 + sc2) + sh2, lp)
        if with_ctx:
            cx = cx + cg1 * (y_ctx @ w_out[l])
            cx = cx + cg2 * hier_moe(rmsnorm(cx, norm2_g[l]) * (1.0 + csc2) + csh2, lp)
    return rmsnorm(lat, final_g).astype(x.dtype)
```

```python
import contextlib
import numpy as np
import concourse.bass as bass
import concourse.mybir as mybir

F32 = mybir.dt.float32
BF16 = mybir.dt.bfloat16
I32 = mybir.dt.int32
AF = mybir.ActivationFunctionType
ALU = mybir.AluOpType
AX = mybir.AxisListType

ENGS = ("pe", "act", "dve", "pool", "sp")
N_DMA_SEM = 8
N_C_SEM = 4


class Buf:
    __slots__ = ("name", "t", "last_w", "reads", "psum")

    def __init__(self, name, t=None, psum=False):
        self.name = name
        self.t = t
        self.psum = psum
        self.last_w = None
        self.reads = []

    def __getitem__(self, idx):
        return self.t[idx]


class Op:
    __slots__ = ("eng", "fn", "waits", "ev", "is_dma", "needed", "stage")


class Prog:
    def __init__(self, nc):
        self.nc = nc
        self.stack = contextlib.ExitStack()
        self.base = self.stack
        self.sems = None
        self.sigval = {(e, r): 0 for e in ENGS for r in range(N_C_SEM)}
        self.barrier = []
        self.val = {}
        self.stage_id = 0
        self.q = {e: [] for e in ENGS}
        self.seen = {e: {} for e in ENGS}
        self.sig_count = {e: 0 for e in ENGS}
        self.dma_count = {e: 0 for e in ENGS}
        self.dma_events = {e: [] for e in ENGS}
        self.nbuf = 0

    def sb(self, shape, dt=F32, name=None):
        self.nbuf += 1
        name = name or f"sb{self.nbuf}"
        t = self.stack.enter_context(self.nc.sbuf_tensor(name, list(shape), dt))
        return Buf(name, t)

    def ps(self, shape, dt=F32, name=None):
        self.nbuf += 1
        name = name or f"ps{self.nbuf}"
        per_bank = 512 if dt == F32 else 1024
        full = self.stack.enter_context(self.nc.psum_tensor(name, [128, per_bank], dt))
        shape = list(shape)
        n = 1
        for d in shape[1:]:
            n *= d
        assert n <= per_bank and shape[0] <= 128
        v = full[0:shape[0], 0:n]
        if len(shape) == 3:
            v = v.rearrange("p (a b) -> p a b", a=shape[1])
        return Buf(name, v, psum=True)

    def dram(self, name, shape, dt=F32, kind="Internal"):
        h = self.nc.dram_tensor(name, list(shape), dt, kind=kind)
        return h.ap()

    def key(self, name):
        return Buf(name)

    def _deps(self, eng, reads, writes):
        evs = []
        for b in reads:
            if b.last_w is not None:
                evs.append(b.last_w)
            if b.psum:
                evs.extend(ev for ev in b.reads if ev[2].eng != eng)
        for b in writes:
            if b.last_w is not None:
                evs.append(b.last_w)
            evs.extend(b.reads)
        return evs

    def _mk_waits(self, eng, evs):
        need = {}
        for (k, v, op) in evs:
            if op.stage != self.stage_id or self.seen[eng].get(k, 0) >= v:
                continue
            if need.get(k, (0, None))[0] < v:
                need[k] = (v, op)
        waits = []
        for k, (v, op) in need.items():
            self.seen[eng][k] = v
            op.needed = True
            waits.append((k, op))
        return waits

    def op(self, eng, name, R=(), W=(), tw=True, **kw):
        reads, writes, track_write = R, W, tw
        fn = (lambda e, name=name, kw=kw: getattr(e, name)(**kw))
        o = Op()
        o.stage = self.stage_id
        o.eng, o.fn, o.is_dma, o.needed = eng, fn, False, False
        o.waits = self._mk_waits(eng, self._deps(eng, reads, writes))
        i = self.sig_count[eng]
        self.sig_count[eng] += 1
        o.ev = (("c", eng, i % N_C_SEM), i // N_C_SEM + 1, o)
        self.q[eng].append(o)
        for b in reads:
            b.reads.append(o.ev)
        if track_write:
            for b in writes:
                b.last_w = o.ev
                b.reads = []
        return o

    def dma(self, eng, out, in_, R=(), W=(), **kw):
        reads, writes = R, W
        o = Op()
        o.stage = self.stage_id
        o.eng, o.is_dma, o.needed = eng, True, True
        i = self.dma_count[eng]
        self.dma_count[eng] += 1
        r = i % N_DMA_SEM
        evs = self._deps(eng, reads, writes)
        if i >= N_DMA_SEM:
            evs.append(self.dma_events[eng][i - N_DMA_SEM])
        o.waits = self._mk_waits(eng, evs)
        o.ev = (("d", eng, r), 16 * (i // N_DMA_SEM + 1), o)
        self.dma_events[eng].append(o.ev)
        custom = kw.pop("custom", None)
        if custom is not None:
            o.fn = custom
        else:
            o.fn = lambda e, out=out, in_=in_, kw=kw: e.dma_start(out=out, in_=in_, **kw)
        self.q[eng].append(o)
        for b in reads:
            b.reads.append(o.ev)
        for b in writes:
            b.last_w = o.ev
            b.reads = []
        return o

    @contextlib.contextmanager
    def stage(self, name=""):
        sub = contextlib.ExitStack()
        self.stack = sub
        self.nstage = getattr(self, "nstage", 0) + 1
        try:
            yield
            with self.nc.named_scope(f"s{self.nstage:02d}_{name}"):
                self.emit()
        finally:
            self.stack = self.base
            sub.close()

    def emit(self):
        nc = self.nc
        if self.sems is None:
            self.sems = {}
            for e in ENGS:
                for r in range(N_C_SEM):
                    self.sems[("c", e, r)] = self.base.enter_context(nc.semaphore(f"c_{e}{r}"))
                for r in range(N_DMA_SEM):
                    self.sems[("d", e, r)] = self.base.enter_context(nc.semaphore(f"d_{e}{r}"))
        sems = self.sems
        nxt = []
        for e in ENGS:
            comp = [o for o in self.q[e] if not o.is_dma and o.ev[1] > 0]
            if comp:
                comp[-1].needed = True
                nxt.append(comp[-1].ev)
            last = {}
            for o in self.q[e]:
                if o.is_dma:
                    last[o.ev[0]] = o.ev
            nxt.extend(last.values())
        val = self.val
        for e in ENGS:
            for o in self.q[e]:
                if o.is_dma:
                    val[id(o)] = o.ev[1]
                elif o.needed:
                    kk = (e, o.ev[0][2])
                    self.sigval[kk] += 1
                    val[id(o)] = self.sigval[kk]
        prologue = {}
        for e in ENGS:
            w = []
            for (k, v, dep) in self.barrier:
                w.append((k, val[id(dep)]))
            prologue[e] = w
        with nc.Block() as block:
            self._emit_block(block, sems, val, prologue)
        self.barrier = nxt
        self.q = {e: [] for e in ENGS}
        self.stage_id += 1

    def _emit_block(self, block, sems, val, prologue):
        getters = {"pe": block.tensor, "act": block.scalar, "dve": block.vector,
                   "pool": block.gpsimd, "sp": block.sync}

        def body(e):
            def f(engine):
                for (k, v) in prologue[e]:
                    engine.wait_ge(sems[k], v)
                for o in self.q[e]:
                    for (k, dep) in o.waits:
                        engine.wait_ge(sems[k], val[id(dep)])
                    ins = o.fn(engine)
                    if o.is_dma:
                        ins.then_inc(sems[o.ev[0]], 16)
                    elif o.needed:
                        ins.then_inc(sems[o.ev[0]], 1)
            return f

        for e in ENGS:
            getters[e](body(e))

    def finish(self):
        with self.stage("final"):
            pass

    def close(self):
        self.stack.close()
from concourse.bass_utils import run_bass_kernel_spmd
T_CTX, T_LAT = 256, 4096
T_ALL = T_CTX + T_LAT
NT = T_ALL // 128
DM = 2048
KC = DM // 128
DPROJ = 7216
EPS = 1e-6
DEPTH = 2
N_EXP, FF = 32, 256
T_HALF = T_LAT // 2

OFF = {}
_o = 0
for _n, _w in (("hy", 1536), ("gla_q", 256), ("gla_k", 256), ("gla_v", 512), ("gla_g", 512), ("gla_r", 32),
               ("ml_q", 512), ("ml_k", 512), ("ml_v", 512), ("ml_o", 512), ("ml_gates", 16),
               ("ret_q", 512), ("ret_k", 512), ("ret_v", 512), ("ret_g", 512)):
    OFF[_n] = _o
    _o += _w
assert _o == DPROJ


def tile_r(t):
    return 1 if t < 2 else 0


def rms_rstd(P, xt, junk, ss, rs, width):
    P.op("act", "activation", out=junk[:], in_=xt[:], func=AF.Square, accum_out=ss[:], R=[xt], W=[junk, ss])
    P.op("dve", "tensor_scalar", out=rs[:], in0=ss[:], scalar1=1.0 / width, scalar2=EPS, op0=ALU.mult, op1=ALU.add,
         R=[ss], W=[rs])
    P.op("act", "activation", out=rs[:], in_=rs[:], func=AF.Sqrt, R=[rs], W=[rs])
    P.op("dve", "reciprocal", out=rs[:], in_=rs[:], R=[rs], W=[rs])


def load_mod_tiles(P, D, l, which, rows=(0, 1), gain=None):
    out = {}
    gb = None
    if gain is not None:
        gb = P.sb([128, DM])
        P.dma("sp", gb[:], gain.partition_broadcast(128), W=[gb])
    for r in rows:
        for ch in which:
            t = P.sb([128, DM])
            P.dma("sp", t[:], D["modd"][l, r, ch * DM:(ch + 1) * DM].partition_broadcast(128), W=[t])
            if ch in (1, 4) and gb is not None:
                P.op("dve", "scalar_tensor_tensor", out=t[:], in0=t[:], scalar=1.0, in1=gb[:], op0=ALU.add, op1=ALU.mult,
                     R=[t, gb], W=[t])
            out[(r, ch)] = t
    return out


def stage_init(P, D):
    with P.stage("init"):
        k = P.key("res_init")
        P.dma("sp", D["res"][0:T_CTX, :], D["ctx"], W=[P.key("st")])
        for i in range(4):
            P.dma("sp", D["res"][T_CTX + i * 1024:T_CTX + (i + 1) * 1024, :], D["x"][i * 1024:(i + 1) * 1024, :], W=[P.key("st")])


def stage_mod(P, D, l):
    with P.stage("mod"):
        cT = P.sb([128, KC, 2])
        sT = P.sb([128, KC, 2])
        P.dma("sp", cT[:, :, 0], D["c"].rearrange("(kc p) -> p kc", p=128), W=[cT], allow_slow_non_contiguous=True)
        P.dma("sp", cT[:, :, 1], D["c_ctx"].rearrange("(kc p) -> p kc", p=128), W=[cT], allow_slow_non_contiguous=True)
        P.op("act", "activation", out=sT[:], in_=cT[:], func=AF.Silu, R=[cT], W=[sT])
        badd = P.sb([2, 6 * DM])
        osb = P.sb([2, 6 * DM])
        for r in range(2):
            P.dma("sp", badd[r:r + 1, :], D["ada_b"][l:l + 1, :], W=[badd])
        wb = [P.sb([128, KC, 512]) for _ in range(2)]
        pm = [P.ps([2, 512]) for _ in range(2)]
        for ng in range(24):
            w = wb[ng % 2]
            P.dma("sp", w[:], D["ada_w"][l, :, ng * 512:(ng + 1) * 512].rearrange("(kc p) n -> p kc n", p=128), W=[w])
            pp = pm[ng % 2]
            for kc in range(KC):
                P.op("pe", "matmul", out=pp[:], lhsT=sT[:, kc, :], rhs=w[:, kc, :], start=(kc == 0), stop=(kc == KC - 1),
                     R=[sT, w], W=[pp], tw=(kc == KC - 1))
            P.op("dve", "tensor_tensor", out=osb[:, ng * 512:(ng + 1) * 512], in0=pp[:], in1=badd[:, ng * 512:(ng + 1) * 512],
                 op=ALU.add, R=[pp, badd], W=[osb])
        P.dma("sp", D["modd"][l], osb[:], R=[osb], W=[P.key("modd")])


def stage_inproj(P, D, l, C):
    with P.stage("inproj"):
        mods = load_mod_tiles(P, D, l, which=(0, 1), gain=D["norm1_g"][l])
        idb = C["idb"]
        SG = 17
        xnT = P.sb([128, KC, SG * 128], BF16)
        xt = [P.sb([128, DM]) for _ in range(2)]
        junk = P.sb([128, DM])
        xn = [P.sb([128, DM], BF16) for _ in range(2)]
        ss = [P.sb([128, 1]) for _ in range(2)]
        rs = [P.sb([128, 1]) for _ in range(2)]
        pt = [P.ps([128, 4, 128], BF16) for _ in range(2)]
        wch = [P.sb([128, KC, 512], BF16) for _ in range(2)]
        po = [P.ps([128, 512]) for _ in range(3)]
        osb = [P.sb([128, 512]) for _ in range(3)]
        pk = P.key("p")
        ncg = (DPROJ + 511) // 512
        for sg in range(NT // SG):
            for ti in range(SG):
                t = sg * SG + ti
                r = tile_r(t)
                x_, xn_, ss_, rs_ = xt[t % 2], xn[t % 2], ss[t % 2], rs[t % 2]
                P.dma("sp", x_[:], D["res"][t * 128:(t + 1) * 128, :], W=[x_])
                rms_rstd(P, x_, junk, ss_, rs_, DM)
                P.op("dve", "scalar_tensor_tensor", out=junk[:], in0=x_[:], scalar=rs_[:, 0:1], in1=mods[(r, 1)][:],
                     op0=ALU.mult, op1=ALU.mult, R=[x_, rs_, mods[(r, 1)]], W=[junk])
                P.op("dve", "tensor_tensor", out=xn_[:], in0=junk[:], in1=mods[(r, 0)][:], op=ALU.add,
                     R=[junk, mods[(r, 0)]], W=[xn_])
                for g in range(4):
                    pp = pt[g % 2]
                    for j in range(4):
                        kc = g * 4 + j
                        P.op("pe", "transpose", out=pp[:, j, :], in_=xn_[:, kc * 128:(kc + 1) * 128], identity=idb[:],
                             R=[xn_, idb], W=[pp])
                    P.op("act", "copy", out=xnT[:, g * 4:(g + 1) * 4, ti * 128:(ti + 1) * 128], in_=pp[:], R=[pp], W=[xnT])
            cnt = 0
            for cg in range(ncg):
                c0 = cg * 512
                cw = min(512, DPROJ - c0)
                w = wch[cg % 2]
                P.dma("pool", w[:, :, 0:cw], D["w_in"][l, :, c0:c0 + cw].rearrange("(kc p) n -> p kc n", p=128), W=[w])
                for ti in range(SG):
                    t = sg * SG + ti
                    pp, ob = po[cnt % 3], osb[cnt % 3]
                    for kc in range(KC):
                        P.op("pe", "matmul", out=pp[:, 0:cw], lhsT=xnT[:, kc, ti * 128:(ti + 1) * 128], rhs=w[:, kc, 0:cw],
                             start=(kc == 0), stop=(kc == KC - 1), R=[xnT, w], W=[pp], tw=(kc == KC - 1))
                    if cnt % 2 == 0:
                        P.op("act", "copy", out=ob[:, 0:cw], in_=pp[:, 0:cw], R=[pp], W=[ob])
                    else:
                        P.op("dve", "tensor_copy", out=ob[:, 0:cw], in_=pp[:, 0:cw], R=[pp], W=[ob])
                    P.dma("sp", D["p"][t * 128:(t + 1) * 128, c0:c0 + cw], ob[:, 0:cw], R=[ob], W=[P.key("st")])
                    cnt += 1


def stage_outproj(P, D, l, C, tiles, gather=False):
    with P.stage("outproj"):
        rows = [0] if gather else sorted(set(tile_r(t) for t in tiles))
        if gather:
            idx = P.sb([128, len(tiles)], I32)
            P.dma("sp", idx[:], D["k_rows"], W=[idx])
        g1 = load_mod_tiles(P, D, l, which=(2,), rows=rows)
        idb = C["idb"]
        wo = P.sb([128, KC, DM], BF16)
        for h in range(4):
            P.dma("pool", wo[:, :, h * 512:(h + 1) * 512],
                  D["w_out"][l, :, h * 512:(h + 1) * 512].rearrange("(kc p) n -> p kc n", p=128), W=[wo])
        yt = [P.sb([128, DM]) for _ in range(2)]
        yb = [P.sb([128, DM], BF16) for _ in range(2)]
        yT = [P.sb([128, KC, 128], BF16) for _ in range(2)]
        xr = [P.sb([128, DM]) for _ in range(2)]
        tmp = [P.sb([128, 512]) for _ in range(2)]
        pt = [P.ps([128, 4, 128], BF16) for _ in range(2)]
        po = [P.ps([128, 512]) for _ in range(2)]
        rk = P.key("res")
        def loads(i):
            t = tiles[i]
            if gather:
                off = bass.IndirectOffsetOnAxis(ap=idx[:, t:t + 1], axis=0)
                for buf, src in ((yt[i % 2], "ymix"), (xr[i % 2], "res")):
                    P.dma("pool", None, None, R=[idx], W=[buf],
                          custom=lambda e, buf=buf, src=src, off=off: e.indirect_dma_start(
                              out=buf[:], out_offset=None, in_=D[src][:, :], in_offset=off))
                return
            P.dma("sp", yt[i % 2][:], D["ymix"][t * 128:(t + 1) * 128, :], W=[yt[i % 2]])
            P.dma("sp", xr[i % 2][:], D["res"][t * 128:(t + 1) * 128, :], W=[xr[i % 2]])

        loads(0)
        for i, t in enumerate(tiles):
            r = 0 if gather else tile_r(t)
            y_, yb_, yT_, x_ = yt[i % 2], yb[i % 2], yT[i % 2], xr[i % 2]
            if i + 1 < len(tiles):
                loads(i + 1)
            P.op("act", "copy", out=yb_[:], in_=y_[:], R=[y_], W=[yb_])
            for g in range(4):
                pp = pt[g % 2]
                for j in range(4):
                    kc = g * 4 + j
                    P.op("pe", "transpose", out=pp[:, j, :], in_=yb_[:, kc * 128:(kc + 1) * 128], identity=idb[:],
                         R=[yb_, idb], W=[pp])
                P.op("act", "copy", out=yT_[:, g * 4:(g + 1) * 4, :], in_=pp[:], R=[pp], W=[yT_])
            for cg in range(4):
                pp, tm = po[cg % 2], tmp[cg % 2]
                for kc in range(KC):
                    P.op("pe", "matmul", out=pp[:], lhsT=yT_[:, kc, :], rhs=wo[:, kc, cg * 512:(cg + 1) * 512],
                         start=(kc == 0), stop=(kc == KC - 1), R=[yT_, wo], W=[pp], tw=(kc == KC - 1))
                P.op("dve", "tensor_tensor", out=tm[:], in0=pp[:], in1=g1[(r, 2)][:, cg * 512:(cg + 1) * 512], op=ALU.mult,
                     R=[pp, g1[(r, 2)]], W=[tm])
                P.op("pool", "tensor_tensor", out=x_[:, cg * 512:(cg + 1) * 512], in0=x_[:, cg * 512:(cg + 1) * 512], in1=tm[:],
                     op=ALU.add, R=[x_, tm], W=[x_])
            P.dma("sp", D["res_h" if gather else "res"][t * 128:(t + 1) * 128, :], x_[:], R=[x_], W=[P.key("st")])


def stage_moe_a(P, D, l, C, tiles, res="res", lat_only=False):
    with P.stage("moe_a"):
        rows = [0] if lat_only else sorted(set(tile_r(t) for t in tiles))
        mods = load_mod_tiles(P, D, l, which=(3, 4), rows=rows, gain=D["norm2_g"][l])
        idf = C["idf"]
        wr = P.sb([128, KC, 36])
        P.dma("sp", wr[:, :, 0:4], D["moe_wg"][l].rearrange("(kc p) n -> p kc n", p=128), W=[wr])
        P.dma("sp", wr[:, :, 4:36], D["moe_we"][l].rearrange("(kc p) n -> p kc n", p=128), W=[wr])
        br = P.sb([128, 36])
        P.dma("sp", br[:, 0:4], D["moe_bg"][l].partition_broadcast(128), W=[br])
        P.dma("sp", br[:, 4:36], D["moe_be"][l].partition_broadcast(128), W=[br])
        xt = [P.sb([128, DM]) for _ in range(2)]
        junk = P.sb([128, DM])
        xn = [P.sb([128, DM]) for _ in range(2)]
        ss = [P.sb([128, 1]) for _ in range(2)]
        rs = [P.sb([128, 1]) for _ in range(2)]
        xTf = [P.sb([128, KC, 128]) for _ in range(2)]
        xTb = [P.sb([128, KC, 128], BF16) for _ in range(2)]
        pt = [P.ps([128, 4, 128]) for _ in range(2)]
        pl = P.ps([128, 36])
        pc = P.ps([32, 128])
        lg = P.sb([128, 36])
        sm = {n: P.sb([128, w]) for n, w in (("gmax", 1), ("ngmax", 1), ("gsum", 1), ("pg", 1), ("gexp", 4), ("gmask", 4),
                                              ("gpen", 4), ("el", 32), ("m1", 1), ("k1", 32), ("el2", 32), ("m2", 1),
                                              ("k2", 32), ("d", 1), ("e", 1), ("w1", 1), ("w2", 1), ("cmb", 32), ("cmb2", 32))}
        cT = [P.sb([32, 128]) for _ in range(2)]
        kx, kc_ = P.key("xn2T"), P.key("combT")
        BIG = 1.0e30
        P.dma("sp", xt[0][:], D[res][tiles[0] * 128:(tiles[0] + 1) * 128, :], W=[xt[0]])
        for i, t in enumerate(tiles):
            r = 0 if lat_only else tile_r(t)
            x_, xn_, ss_, rs_, xTf_, xTb_ = xt[i % 2], xn[i % 2], ss[i % 2], rs[i % 2], xTf[i % 2], xTb[i % 2]
            if i + 1 < len(tiles):
                tn = tiles[i + 1]
                P.dma("sp", xt[(i + 1) % 2][:], D[res][tn * 128:(tn + 1) * 128, :], W=[xt[(i + 1) % 2]])
            rms_rstd(P, x_, junk, ss_, rs_, DM)
            P.op("dve", "scalar_tensor_tensor", out=junk[:], in0=x_[:], scalar=rs_[:, 0:1], in1=mods[(r, 4)][:],
                 op0=ALU.mult, op1=ALU.mult, R=[x_, rs_, mods[(r, 4)]], W=[junk])
            P.op("pool", "tensor_tensor", out=xn_[:], in0=junk[:], in1=mods[(r, 3)][:], op=ALU.add,
                 R=[junk, mods[(r, 3)]], W=[xn_])
            for g in range(4):
                pp = pt[g % 2]
                for j in range(4):
                    kc = g * 4 + j
                    P.op("pe", "transpose", out=pp[:, j, :], in_=xn_[:, kc * 128:(kc + 1) * 128], identity=idf[:],
                         R=[xn_, idf], W=[pp])
                P.op("act", "copy", out=xTf_[:, g * 4:(g + 1) * 4, :], in_=pp[:], R=[pp], W=[xTf_])
            P.op("dve", "tensor_copy", out=xTb_[:], in_=xTf_[:], R=[xTf_], W=[xTb_])
            P.dma("sp", D["xn2T"][:, :, t * 128:(t + 1) * 128], xTb_[:], R=[xTb_], W=[P.key("st")])
            for kc in range(KC):
                P.op("pe", "matmul", out=pl[:], lhsT=xTf_[:, kc, :], rhs=wr[:, kc, :], start=(kc == 0), stop=(kc == KC - 1),
                     R=[xTf_, wr], W=[pl], tw=(kc == KC - 1))
            P.op("dve", "tensor_tensor", out=lg[:], in0=pl[:], in1=br[:], op=ALU.add, R=[pl, br], W=[lg])
            s = sm
            V = lambda name, **kw: P.op("dve", name, **kw)
            V("reduce_max", out=s["gmax"][:], in_=lg[:, 0:4], axis=AX.X, R=[lg], W=[s["gmax"]])
            V("tensor_scalar", out=s["ngmax"][:], in0=s["gmax"][:], scalar1=-1.0, scalar2=None, op0=ALU.mult,
              R=[s["gmax"]], W=[s["ngmax"]])
            P.op("act", "activation", out=s["gexp"][:], in_=lg[:, 0:4], func=AF.Exp, bias=s["ngmax"][:, 0:1],
                 accum_out=s["gsum"][:], R=[lg, s["ngmax"]], W=[s["gexp"], s["gsum"]])
            V("reciprocal", out=s["pg"][:], in_=s["gsum"][:], R=[s["gsum"]], W=[s["pg"]])
            V("tensor_scalar", out=s["gmask"][:], in0=lg[:, 0:4], scalar1=s["gmax"][:, 0:1], scalar2=None, op0=ALU.is_ge,
              R=[lg, s["gmax"]], W=[s["gmask"]])
            V("tensor_scalar", out=s["gpen"][:], in0=s["gmask"][:], scalar1=-1.0, scalar2=BIG, op0=ALU.add, op1=ALU.mult,
              R=[s["gmask"]], W=[s["gpen"]])
            V("tensor_tensor", out=s["el"][:].rearrange("p (g e) -> p g e", g=4),
              in0=lg[:, 4:36].rearrange("p (g e) -> p g e", g=4),
              in1=s["gpen"][:].unsqueeze(2).to_broadcast([128, 4, 8]), op=ALU.add, R=[lg, s["gpen"]], W=[s["el"]])
            V("reduce_max", out=s["m1"][:], in_=s["el"][:], axis=AX.X, R=[s["el"]], W=[s["m1"]])
            V("tensor_scalar", out=s["k1"][:], in0=s["el"][:], scalar1=s["m1"][:, 0:1], scalar2=None, op0=ALU.is_ge,
              R=[s["el"], s["m1"]], W=[s["k1"]])
            V("scalar_tensor_tensor", out=s["el2"][:], in0=s["k1"][:], scalar=-BIG, in1=s["el"][:], op0=ALU.mult, op1=ALU.add,
              R=[s["k1"], s["el"]], W=[s["el2"]])
            V("reduce_max", out=s["m2"][:], in_=s["el2"][:], axis=AX.X, R=[s["el2"]], W=[s["m2"]])
            V("tensor_scalar", out=s["k2"][:], in0=s["el2"][:], scalar1=s["m2"][:, 0:1], scalar2=None, op0=ALU.is_ge,
              R=[s["el2"], s["m2"]], W=[s["k2"]])
            V("tensor_tensor", out=s["d"][:], in0=s["m2"][:], in1=s["m1"][:], op=ALU.subtract, R=[s["m1"], s["m2"]], W=[s["d"]])
            P.op("act", "activation", out=s["e"][:], in_=s["d"][:], func=AF.Exp, R=[s["d"]], W=[s["e"]])
            V("tensor_scalar", out=s["w1"][:], in0=s["e"][:], scalar1=1.0, scalar2=None, op0=ALU.add, R=[s["e"]], W=[s["w1"]])
            V("reciprocal", out=s["w1"][:], in_=s["w1"][:], R=[s["w1"]], W=[s["w1"]])
            V("tensor_tensor", out=s["w2"][:], in0=s["e"][:], in1=s["w1"][:], op=ALU.mult, R=[s["e"], s["w1"]], W=[s["w2"]])
            V("tensor_tensor", out=s["w1"][:], in0=s["w1"][:], in1=s["pg"][:], op=ALU.mult, R=[s["w1"], s["pg"]], W=[s["w1"]])
            V("tensor_tensor", out=s["w2"][:], in0=s["w2"][:], in1=s["pg"][:], op=ALU.mult, R=[s["w2"], s["pg"]], W=[s["w2"]])
            V("tensor_scalar", out=s["cmb"][:], in0=s["k1"][:], scalar1=s["w1"][:, 0:1], scalar2=None, op0=ALU.mult,
              R=[s["k1"], s["w1"]], W=[s["cmb"]])
            V("scalar_tensor_tensor", out=s["cmb2"][:], in0=s["k2"][:], scalar=s["w2"][:, 0:1], in1=s["cmb"][:],
              op0=ALU.mult, op1=ALU.add, R=[s["k2"], s["w2"], s["cmb"]], W=[s["cmb2"]])
            P.op("pe", "transpose", out=pc[:], in_=s["cmb2"][:], identity=idf[:], R=[s["cmb2"], idf], W=[pc])
            c_ = cT[i % 2]
            P.op("act", "copy", out=c_[:], in_=pc[:], R=[pc], W=[c_])
            P.dma("sp", D["combT"][:, t * 128:(t + 1) * 128], c_[:], R=[c_], W=[P.key("st")])


def stage_moe_b(P, D, l, C, groups, res="res", lat_only=False):
    with P.stage("moe_b"):
        GT = max(len(g) for g in groups)
        NTOK = GT * 128
        xT = P.sb([128, KC, NTOK], BF16)
        yacc = [P.sb([128, DM]) for _ in range(GT)]
        cb = [P.sb([128, NTOK]) for _ in range(2)]
        actT = [P.sb([128, 2, 2, NTOK], BF16) for _ in range(2)]
        w13 = [P.sb([128, 2, KC, FF], BF16) for _ in range(2)]
        w2 = [P.sb([128, 2, DM], BF16) for _ in range(4)]
        sa = [P.sb([128, 512]) for _ in range(2)]
        sab = sa
        pa = [P.ps([128, 512]) for _ in range(2)]
        pb = [P.ps([128, 512]) for _ in range(2)]
        NPO = 3
        po = [P.ps([128, 512]) for _ in range(NPO)]
        gt = P.sb([128, DM])
        xr = P.sb([128, DM])
        rk = P.key("res")
        cnt = {"p1": 0, "p2": 0}

        def p1_block(e, slot, j, h0, hw, fc):
            wa, c_ = w13[e % 2], cb[e % 2]
            k = cnt["p1"]
            cnt["p1"] += 1
            pa_, pb_, sa_, sab_ = pa[k % 2], pb[k % 2], sa[k % 2], sab[k % 2]
            for kc in range(KC):
                P.op("pe", "matmul", out=pa_[:, 0:hw], lhsT=wa[:, 0, kc, fc * 128:(fc + 1) * 128], rhs=xT[:, kc, h0:h0 + hw],
                     start=(kc == 0), stop=(kc == KC - 1), R=[wa, xT], W=[pa_], tw=(kc == KC - 1))
            for kc in range(KC):
                P.op("pe", "matmul", out=pb_[:, 0:hw], lhsT=wa[:, 1, kc, fc * 128:(fc + 1) * 128], rhs=xT[:, kc, h0:h0 + hw],
                     start=(kc == 0), stop=(kc == KC - 1), R=[wa, xT], W=[pb_], tw=(kc == KC - 1))
            P.op("act", "activation", out=sa_[:, 0:hw], in_=pa_[:, 0:hw], func=AF.Silu, R=[pa_], W=[sa_])
            P.op("dve", "tensor_tensor", out=sab_[:, 0:hw], in0=pb_[:, 0:hw], in1=sa_[:, 0:hw], op=ALU.mult, R=[pb_, sa_], W=[sab_])
            P.op("pool", "tensor_tensor", out=actT[slot][:, j, fc, h0:h0 + hw], in0=sab_[:, 0:hw], in1=c_[:, h0:h0 + hw],
                 op=ALU.mult, R=[sab_, c_], W=[actT[slot]])

        def p2_item(pair, slot, ti, cg, first):
            k = cnt["p2"]
            cnt["p2"] += 1
            pp = po[k % len(po)]
            n = 0
            for j, e in enumerate(pair):
                for fc in range(2):
                    P.op("pe", "matmul", out=pp[:], lhsT=actT[slot][:, j, fc, ti * 128:(ti + 1) * 128],
                         rhs=w2[e % 4][:, fc, cg * 512:(cg + 1) * 512], start=(n == 0), stop=(n == 3),
                         R=[actT[slot], w2[e % 4]], W=[pp], tw=(n == 3))
                    n += 1
            dst = yacc[ti][:, cg * 512:(cg + 1) * 512]
            if first:
                P.op("dve", "tensor_copy", out=dst, in_=pp[:], R=[pp], W=[yacc[ti]])
            else:
                P.op("dve", "tensor_tensor", out=dst, in0=pp[:], in1=dst, op=ALU.add, R=[pp, yacc[ti]], W=[yacc[ti]])

        for grp in groups:
            nt = len(grp)
            t0 = grp[0]
            assert grp == list(range(t0, t0 + nt))
            ntok = nt * 128
            r = 0 if lat_only else tile_r(t0)
            P.dma("sp", xT[:, :, 0:ntok], D["xn2T"][:, :, t0 * 128:t0 * 128 + ntok], W=[xT])
            halves = [(h0, min(512, ntok - h0)) for h0 in range(0, ntok, 512)]
            prev = None
            for k in range(N_EXP // 2):
                pair = (2 * k, 2 * k + 1)
                slot = k % 2
                blocks = []
                for j, e in enumerate(pair):
                    wa, wb2, c_ = w13[e % 2], w2[e % 4], cb[e % 2]
                    P.dma("pool", wa[:, 0], D["moe_w1"][l, e // 8, e % 8].rearrange("(kc p) f -> p kc f", p=128), W=[wa])
                    P.dma("pool", wa[:, 1], D["moe_w3"][l, e // 8, e % 8].rearrange("(kc p) f -> p kc f", p=128), W=[wa])
                    P.dma("pool", wb2[:], D["moe_w2"][l, e // 8, e % 8].rearrange("(fc p) d -> p fc d", p=128), W=[wb2])
                    P.dma("sp", c_[:, 0:ntok], D["combT"][e, t0 * 128:t0 * 128 + ntok].partition_broadcast(128), W=[c_])
                    for (h0, hw) in halves:
                        for fc in range(2):
                            blocks.append((e, slot, j, h0, hw, fc))
                items = []
                if prev is not None:
                    items = [(prev[0], prev[1], ti, cg, prev[2]) for ti in range(nt) for cg in range(4)]
                per = (len(items) + len(blocks) - 1) // len(blocks) if items else 0
                for bi, blk in enumerate(blocks):
                    p1_block(*blk)
                    for it in items[bi * per:(bi + 1) * per]:
                        p2_item(*it)
                prev = (pair, slot, k == 0)
            for ti in range(nt):
                for cg in range(4):
                    p2_item(prev[0], prev[1], ti, cg, prev[2])
            P.dma("sp", gt[:], D["modd"][l, r, 5 * DM:6 * DM].partition_broadcast(128), W=[gt])
            for ti, t in enumerate(grp):
                P.dma("sp", xr[:], D[res][t * 128:(t + 1) * 128, :], W=[xr])
                P.op("dve", "tensor_tensor", out=yacc[ti][:], in0=yacc[ti][:], in1=gt[:], op=ALU.mult, R=[yacc[ti], gt], W=[yacc[ti]])
                P.op("dve", "tensor_tensor", out=xr[:], in0=xr[:], in1=yacc[ti][:], op=ALU.add, R=[xr, yacc[ti]], W=[xr])
                P.dma("sp", D[res][t * 128:(t + 1) * 128, :], xr[:], R=[xr], W=[P.key("st")])


def stage_final(P, D):
    with P.stage("final_norm"):
        gb = P.sb([128, DM])
        P.dma("sp", gb[:], D["final_g"].partition_broadcast(128), W=[gb])
        xt = [P.sb([128, DM]) for _ in range(2)]
        ot = [P.sb([128, DM]) for _ in range(2)]
        junk = P.sb([128, DM])
        ss = [P.sb([128, 1]) for _ in range(2)]
        rs = [P.sb([128, 1]) for _ in range(2)]
        n = T_HALF // 128
        P.dma("sp", xt[0][:], D["res_h"][0:128, :], W=[xt[0]])
        for i in range(n):
            x_, o_, ss_, rs_ = xt[i % 2], ot[i % 2], ss[i % 2], rs[i % 2]
            if i + 1 < n:
                P.dma("sp", xt[(i + 1) % 2][:], D["res_h"][(i + 1) * 128:(i + 2) * 128, :], W=[xt[(i + 1) % 2]])
            rms_rstd(P, x_, junk, ss_, rs_, DM)
            P.op("dve", "scalar_tensor_tensor", out=o_[:], in0=x_[:], scalar=rs_[:, 0:1], in1=gb[:], op0=ALU.mult, op1=ALU.mult,
                 R=[x_, rs_, gb], W=[o_])
            P.dma("sp", D["out"][i * 128:(i + 1) * 128, :], o_[:], R=[o_], W=[P.key("st")])
LA_CFG = {"gla": dict(dk=64, ycol=512, g="gla_g", ng="gla_norm_g"),
          "ml": dict(dk=128, ycol=1024, g="ml_o", ng="ml_norm_g"),
          "ret": dict(dk=128, ycol=1536, g="ret_g", ng="ret_norm_g")}


def _interleave(gens):
    active = [g for g in gens if g is not None]
    while active:
        for g in list(active):
            try:
                next(g)
            except StopIteration:
                active.remove(g)


def stage_linattn(P, D, l, C, kind, dr, with_ctx):
    cfg = LA_CFG[kind]
    H, dk, dv, dvp = 4, cfg["dk"], 128, 128
    HK = H * dk
    qo, ko, vo, go = OFF[kind + "_q"], OFF[kind + "_k"], OFF[kind + "_v"], OFF[cfg["g"]]
    qscale = dk ** -0.5 if kind in ("gla", "ret") else 1.0
    kscale = dk ** -0.5 if kind == "ml" else 1.0
    idb, idf = C["idb"], C["idf"]
    order = ([0, 1] + list(range(2, NT))) if dr == 0 else ([1, 0] + list(range(NT - 1, 1, -1)))
    with P.stage(f"la_{kind}{dr}"):
        two = lambda shape, dt=F32: [P.sb(shape, dt) for _ in range(2)]
        tri = P.sb([128, 128])
        ones = P.sb([128, 128])
        P.dma("sp", tri[:], D["k_tri"][dr], W=[tri])
        P.dma("sp", ones[:], D["k_tri"][2], W=[ones])
        S = P.sb([dk, H, dvp])
        Sb = P.sb([dk, H, dvp], BF16)
        P.op("dve", "memset", ap=S[:], constant=0.0, W=[S])
        P.op("dve", "memset", ap=Sb[:], constant=0.0, W=[Sb])
        Vb = two([128, H, dvp], BF16)
        if kind == "ml":
            onesb = P.sb([128, 1], BF16)
            P.op("dve", "memset", ap=onesb[:], constant=1.0, W=[onesb])
            nS = P.sb([dk, H])
            nSb = P.sb([dk, H], BF16)
            P.op("dve", "memset", ap=nS[:], constant=0.0, W=[nS])
            P.op("dve", "memset", ap=nSb[:], constant=0.0, W=[nSb])
            pD = P.ps([128, 32])
        LA = two([128, HK])
        if kind == "gla":
            WA = P.sb([32, 256])
            P.op("dve", "memset", ap=WA[:], constant=0.0, W=[WA])
            P.dma("sp", WA[dr * 16:(dr + 1) * 16, :], D["gla_wa2"][l, dr], W=[WA])
            bab = P.sb([128, 256])
            P.dma("sp", bab[:], D["gla_ba"][l, dr].partition_broadcast(128), W=[bab])
            Rt = two([128, 32])
            RT = two([32, 128])
        if kind == "ml":
            bi = P.sb([128, 4])
            bf = P.sb([128, 4])
            P.dma("sp", bi[:], D["ml_gate_b"][l, dr, 0].partition_broadcast(128), W=[bi])
            P.dma("sp", bf[:], D["ml_gate_b"][l, dr, 1].partition_broadcast(128), W=[bf])
            Gt = two([128, 16])
            lf = two([128, 4])
            eig = two([128, 4])
        if kind == "ret":
            rd = P.sb([128, 4])
            P.dma("sp", rd[:], D["ret_decay"][l, dr].partition_broadcast(128), W=[rd])
            P.op("act", "activation", out=rd[:], in_=rd[:], func=AF.Exp, scale=-1.0, R=[rd], W=[rd])
            P.op("act", "activation", out=rd[:], in_=rd[:], func=AF.Ln, bias=1.0, R=[rd], W=[rd])
            P.op("dve", "tensor_scalar", out=LA[0][:].rearrange("p (h d) -> p h d", h=H),
                 in0=rd[:].unsqueeze(2).to_broadcast([128, H, dk]), scalar1=-1.0, scalar2=None, op0=ALU.mult, R=[rd], W=[LA[0]])
            cs = two([128, 64])
            sn = two([128, 64])
            rtq = [P.sb([128, H, 64]) for _ in range(4)]
            rtk = [P.sb([128, H, 64]) for _ in range(4)]
        Qt, Kt, Vt = two([128, HK]), two([128, HK]), two([128, 512])
        BCs, EQ, EK, ES = two([128, HK]), two([128, HK]), two([128, HK]), two([128, HK])
        Qin, Kin, Kst = two([128, HK], BF16), two([128, HK], BF16), two([128, HK], BF16)
        tmpk = two([128, HK])
        A = two([dk, H])
        QKT = two([dk, 2 * H, 128], BF16)
        atts = P.sb([128, H, 128], BF16)
        pBC, pBT = P.ps([128, HK]), P.ps([128, HK])
        pA = P.ps([128, 160])
        pT = P.ps([dk, 2 * H, 128], BF16)
        pAtt = P.ps([128, H, 128])
        pO = P.ps([128, H, dvp])
        pU = P.ps([dk, H, dvp])
        Osb = two([128, H, 128])
        if kind == "ml":
            dn = P.sb([128, H])
        if dr == 1:
            Oprev = two([128, H, 128])
            Gg = two([128, 512])
            gnb = P.sb([128, 512])
            P.dma("sp", gnb[:], D[cfg["ng"]][l].partition_broadcast(128), W=[gnb])
            sq = P.sb([128, H, 128])
            ssq = P.sb([128, H])
        n_it = len(order)
        hoist = {"done": False}

        def flags(it):
            t = order[it]
            is_ctx = t < 2
            return t, is_ctx, (with_ctx or not is_ctx)

        def loads(it):
            t, is_ctx, need_out = flags(it)
            b = it % 2
            rows = slice(t * 128, (t + 1) * 128)
            P.dma("sp", Qt[b][:], D["p"][rows, qo:qo + HK], W=[Qt[b]])
            P.dma("sp", Kt[b][:], D["p"][rows, ko:ko + HK], W=[Kt[b]])
            P.dma("sp", Vt[b][:], D["p"][rows, vo:vo + 512], W=[Vt[b]])
            if kind == "gla":
                P.dma("sp", Rt[b][:], D["p"][rows, OFF["gla_r"]:OFF["gla_r"] + 32], W=[Rt[b]])
            if kind == "ml":
                P.dma("sp", Gt[b][:], D["p"][rows, OFF["ml_gates"]:OFF["ml_gates"] + 16], W=[Gt[b]])
            if kind == "ret" and not is_ctx:
                lr = slice((t - 2) * 128, (t - 1) * 128)
                P.dma("sp", cs[b][:], D["k_cos"][lr, :], W=[cs[b]])
                P.dma("sp", sn[b][:], D["k_sin"][lr, :], W=[sn[b]])

        def loads_post(it):
            t, is_ctx, need_out = flags(it)
            b = it % 2
            rows = slice(t * 128, (t + 1) * 128)
            if dr == 1 and need_out:
                P.dma("sp", Oprev[b][:], D["oacc"][rows, :].rearrange("p (h d) -> p h d", h=H), W=[Oprev[b]])
                P.dma("sp", Gg[b][:], D["p"][rows, go:go + 512], W=[Gg[b]])

        def prep(it):
            t, is_ctx, need_out = flags(it)
            b = it % 2
            c = 0 if kind == "ret" else b
            Q, K, V = Qt[b], Kt[b], Vt[b]
            LA_, BCs_, EQ_, EK_, ES_, A_ = LA[c], BCs[c], EQ[c], EK[c], ES[c], A[c]
            if kind == "gla":
                R_, RT_ = Rt[b], RT[b]
                P.op("pe", "transpose", out=pA[0:32, 32:160], in_=R_[:], identity=idf[:], R=[R_, idf], W=[pA])
                yield
                P.op("act", "copy", out=RT_[:], in_=pA[0:32, 32:160], R=[pA], W=[RT_])
                yield
                P.op("pe", "matmul", out=pBC[:], lhsT=RT_[:], rhs=WA[:], start=True, stop=True, R=[RT_, WA], W=[pBC])
                yield
                P.op("dve", "tensor_tensor", out=LA_[:], in0=pBC[:], in1=bab[:], op=ALU.add, R=[pBC, bab], W=[LA_])
                yield
                P.op("act", "activation", out=LA_[:], in_=LA_[:], func=AF.Exp, scale=-1.0, R=[LA_], W=[LA_])
                yield
                P.op("act", "activation", out=LA_[:], in_=LA_[:], func=AF.Ln, bias=1.0, R=[LA_], W=[LA_])
                yield
                P.op("dve", "tensor_scalar", out=LA_[:], in0=LA_[:], scalar1=-1.0 / 16.0, scalar2=None, op0=ALU.mult, R=[LA_], W=[LA_])
                yield
            if kind == "ml":
                G4, eig_, lf_ = Gt[b], eig[b], lf[b]
                P.op("dve", "tensor_tensor", out=eig_[:], in0=G4[:, dr * 8:dr * 8 + 4], in1=bi[:], op=ALU.add, R=[G4, bi], W=[eig_])
                yield
                P.op("act", "activation", out=eig_[:], in_=eig_[:], func=AF.Exp, R=[eig_], W=[eig_])
                yield
                P.op("dve", "tensor_tensor", out=lf_[:], in0=G4[:, dr * 8 + 4:dr * 8 + 8], in1=bf[:], op=ALU.add, R=[G4, bf], W=[lf_])
                yield
                P.op("act", "activation", out=lf_[:], in_=lf_[:], func=AF.Exp, scale=-1.0, R=[lf_], W=[lf_])
                yield
                P.op("act", "activation", out=lf_[:], in_=lf_[:], func=AF.Ln, bias=1.0, R=[lf_], W=[lf_])
                yield
                P.op("dve", "tensor_scalar", out=LA_[:].rearrange("p (h d) -> p h d", h=H),
                     in0=lf_[:].unsqueeze(2).to_broadcast([128, H, dk]), scalar1=-1.0, scalar2=None, op0=ALU.mult, R=[lf_], W=[LA_])
                yield
            if not (kind == "ret" and hoist["done"]):
                P.op("pe", "matmul", out=pBC[:], lhsT=tri[:], rhs=LA_[:], start=True, stop=True, R=[tri, LA_], W=[pBC])
                P.op("pe", "matmul", out=pBT[:], lhsT=ones[:], rhs=LA_[:], start=True, stop=True, R=[ones, LA_], W=[pBT])
                for h in range(H):
                    P.op("pe", "matmul", out=pA[0:dk, h:h + 1], lhsT=LA_[:, h * dk:(h + 1) * dk], rhs=ones[:, 0:1], start=True, stop=True,
                         R=[LA_, ones], W=[pA])
                yield
                P.op("act", "copy", out=BCs_[:], in_=pBC[:], R=[pBC], W=[BCs_])
                yield
                P.op("act", "activation", out=EQ_[:], in_=BCs_[:], func=AF.Exp, R=[BCs_], W=[EQ_])
                yield
                P.op("dve", "tensor_tensor", out=ES_[:], in0=pBT[:], in1=BCs_[:], op=ALU.subtract, R=[pBT, BCs_], W=[ES_])
                yield
                P.op("act", "activation", out=EK_[:], in_=BCs_[:], func=AF.Exp, scale=-1.0, R=[BCs_], W=[EK_])
                yield
                P.op("act", "activation", out=ES_[:], in_=ES_[:], func=AF.Exp, R=[ES_], W=[ES_])
                yield
                P.op("act", "activation", out=A_[:], in_=pA[0:dk, 0:H], func=AF.Exp, R=[pA], W=[A_])
                yield
                hoist["done"] = True
            if kind == "ret" and not is_ctx:
                c_, s_ = cs[b], sn[b]
                cb_ = c_[:].unsqueeze(1).to_broadcast([128, H, 64])
                sb_ = s_[:].unsqueeze(1).to_broadcast([128, H, 64])
                for X, eng, rt in ((Q, "dve", rtq), (K, "pool", rtk)):
                    X3 = X[:].rearrange("p (h d) -> p h d", h=H)
                    a1, a2 = X3[:, :, 0:64], X3[:, :, 64:128]
                    t1, t2, t3, t4 = rt
                    P.op(eng, "tensor_tensor", out=t1[:], in0=a1, in1=cb_, op=ALU.mult, R=[X, c_], W=[t1])
                    P.op(eng, "tensor_tensor", out=t2[:], in0=a2, in1=sb_, op=ALU.mult, R=[X, s_], W=[t2])
                    P.op(eng, "tensor_tensor", out=t3[:], in0=a1, in1=sb_, op=ALU.mult, R=[X, s_], W=[t3])
                    P.op(eng, "tensor_tensor", out=t4[:], in0=a2, in1=cb_, op=ALU.mult, R=[X, c_], W=[t4])
                    yield
                    P.op(eng, "tensor_tensor", out=a1, in0=t1[:], in1=t2[:], op=ALU.subtract, R=[t1, t2], W=[X])
                    P.op(eng, "tensor_tensor", out=a2, in0=t3[:], in1=t4[:], op=ALU.add, R=[t3, t4], W=[X])
                    yield
            P.op("dve", "scalar_tensor_tensor", out=Qin[b][:], in0=Q[:], scalar=qscale, in1=EQ_[:], op0=ALU.mult, op1=ALU.mult,
                 R=[Q, EQ_], W=[Qin[b]])
            yield
            if kind == "ml":
                e3 = eig[b][:].unsqueeze(2).to_broadcast([128, H, dk])
                P.op("dve", "scalar_tensor_tensor", out=tmpk[0][:], in0=K[:], scalar=kscale, in1=EK_[:], op0=ALU.mult, op1=ALU.mult,
                     R=[K, EK_], W=[tmpk[0]])
                P.op("dve", "tensor_tensor", out=Kin[b][:].rearrange("p (h d) -> p h d", h=H),
                     in0=tmpk[0][:].rearrange("p (h d) -> p h d", h=H), in1=e3, op=ALU.mult, R=[tmpk[0], eig[b]], W=[Kin[b]])
                yield
                P.op("dve", "scalar_tensor_tensor", out=tmpk[1][:], in0=K[:], scalar=kscale, in1=ES_[:], op0=ALU.mult, op1=ALU.mult,
                     R=[K, ES_], W=[tmpk[1]])
                P.op("dve", "tensor_tensor", out=Kst[b][:].rearrange("p (h d) -> p h d", h=H),
                     in0=tmpk[1][:].rearrange("p (h d) -> p h d", h=H), in1=e3, op=ALU.mult, R=[tmpk[1], eig[b]], W=[Kst[b]])
                yield
            else:
                P.op("dve", "tensor_tensor", out=Kin[b][:], in0=K[:], in1=EK_[:], op=ALU.mult, R=[K, EK_], W=[Kin[b]])
                yield
                P.op("dve", "tensor_tensor", out=Kst[b][:], in0=K[:], in1=ES_[:], op=ALU.mult, R=[K, ES_], W=[Kst[b]])
                yield
            P.op("act", "copy", out=Vb[b][:], in_=V[:].rearrange("p (h d) -> p h d", h=H), R=[V], W=[Vb[b]])
            yield
            if need_out:
                for h in range(H):
                    P.op("pe", "transpose", out=pT[:, h, :], in_=Qin[b][:, h * dk:(h + 1) * dk], identity=idb[:], R=[Qin[b], idb], W=[pT])
                    P.op("pe", "transpose", out=pT[:, H + h, :], in_=Kin[b][:, h * dk:(h + 1) * dk], identity=idb[:], R=[Kin[b], idb], W=[pT])
                yield
                P.op("act", "copy", out=QKT[b][:], in_=pT[:], R=[pT], W=[QKT[b]])
                yield

        def heads(it):
            t, is_ctx, need_out = flags(it)
            b = it % 2
            c = 0 if kind == "ret" else b
            rows = slice(t * 128, (t + 1) * 128)
            QKT_, Vb_, Kst_, A_ = QKT[b], Vb[b], Kst[b], A[c]
            O_ = Osb[b]
            if need_out:
                for h in range(H):
                    P.op("pe", "matmul", out=pAtt[:, h, :], lhsT=QKT_[:, H + h, :], rhs=QKT_[:, h, :], start=True, stop=True, R=[QKT_], W=[pAtt])
                yield
                P.op("dve", "tensor_tensor", out=atts[:], in0=pAtt[:], in1=tri[:].unsqueeze(1).to_broadcast([128, H, 128]), op=ALU.mult,
                     R=[pAtt, tri], W=[atts])
                yield
                for h in range(H):
                    P.op("pe", "matmul", out=pO[:, h, :], lhsT=atts[:, h, :], rhs=Vb_[:, h, :], start=True, stop=False,
                         R=[atts, Vb_], W=[pO], tw=False)
                    P.op("pe", "matmul", out=pO[:, h, :], lhsT=QKT_[:, h, :], rhs=Sb[:, h, :], start=False, stop=True,
                         R=[QKT_, Sb], W=[pO])
                yield
                if kind == "ml":
                    for h in range(H):
                        P.op("pe", "matmul", out=pD[:, h:h + 1], lhsT=atts[:, h, :], rhs=onesb[:, 0:1], start=True, stop=False,
                             R=[atts, onesb], W=[pD], tw=False)
                        P.op("pe", "matmul", out=pD[:, h:h + 1], lhsT=QKT_[:, h, :], rhs=nSb[:, h:h + 1], start=False, stop=True,
                             R=[QKT_, nSb], W=[pD])
                    yield
            for h in range(H):
                P.op("pe", "matmul", out=pU[:, h, :], lhsT=Kst_[:, h * dk:(h + 1) * dk], rhs=Vb_[:, h, :], start=True, stop=True,
                     R=[Kst_, Vb_], W=[pU])
            yield
            if kind == "ml":
                for h in range(H):
                    P.op("pe", "matmul", out=pD[0:dk, 8 + h:9 + h], lhsT=Kst_[:, h * dk:(h + 1) * dk], rhs=onesb[:, 0:1], start=True, stop=True,
                         R=[Kst_, onesb], W=[pD])
                yield
            if need_out:
                if kind == "ml":
                    P.op("act", "activation", out=dn[:], in_=pD[:, 0:4], func=AF.Abs, R=[pD], W=[dn])
                    yield
                    P.op("dve", "tensor_scalar_max", out=dn[:], in0=dn[:], scalar1=1.0, R=[dn], W=[dn])
                    P.op("dve", "reciprocal", out=dn[:], in_=dn[:], R=[dn], W=[dn])
                    P.op("dve", "tensor_tensor", out=O_[:], in0=pO[:], in1=dn[:].unsqueeze(2).to_broadcast([128, H, 128]), op=ALU.mult,
                         R=[pO, dn], W=[O_])
                    yield
                else:
                    P.op("act", "copy", out=O_[:], in_=pO[:], R=[pO], W=[O_])
                    yield
            P.op("dve", "tensor_tensor", out=S[:], in0=S[:], in1=A_[:].unsqueeze(2).to_broadcast([dk, H, dvp]), op=ALU.mult, R=[S, A_], W=[S])
            P.op("dve", "tensor_tensor", out=S[:], in0=pU[:], in1=S[:], op=ALU.add, R=[pU, S], W=[S])
            yield
            P.op("act", "copy", out=Sb[:], in_=S[:], R=[S], W=[Sb])
            yield
            if kind == "ml":
                P.op("dve", "tensor_tensor", out=nS[:], in0=nS[:], in1=A_[:], op=ALU.mult, R=[nS, A_], W=[nS])
                P.op("dve", "tensor_tensor", out=nS[:], in0=pD[0:dk, 8:12], in1=nS[:], op=ALU.add, R=[pD, nS], W=[nS])
                yield
                P.op("act", "copy", out=nSb[:], in_=nS[:], R=[nS], W=[nSb])
                yield
            if not need_out:
                return
            if dr == 0:
                P.dma("sp", D["oacc"][rows, :].rearrange("p (h d) -> p h d", h=H), O_[:], R=[O_], W=[P.key("st")])
                return
            Op_, G_ = Oprev[b], Gg[b]
            P.op("dve", "tensor_tensor", out=O_[:], in0=O_[:], in1=Op_[:], op=ALU.add, R=[O_, Op_], W=[O_])
            yield
            if kind == "ml":
                P.op("act", "activation", out=G_[:], in_=G_[:], func=AF.Sigmoid, R=[G_], W=[G_])
                yield
                P.op("dve", "tensor_tensor", out=O_[:], in0=O_[:], in1=G_[:].rearrange("p (h d) -> p h d", h=H), op=ALU.mult,
                     R=[O_, G_], W=[O_])
                yield
            else:
                P.op("act", "activation", out=G_[:], in_=G_[:], func=AF.Silu, R=[G_], W=[G_])
                yield
            P.op("dve", "tensor_tensor", out=sq[:], in0=O_[:], in1=O_[:], op=ALU.mult, R=[O_], W=[sq])
            P.op("dve", "reduce_sum", out=ssq[:], in_=sq[:], axis=AX.X, R=[sq], W=[ssq])
            P.op("dve", "tensor_scalar", out=ssq[:], in0=ssq[:], scalar1=1.0 / 128, scalar2=EPS, op0=ALU.mult, op1=ALU.add,
                 R=[ssq], W=[ssq])
            yield
            P.op("act", "activation", out=ssq[:], in_=ssq[:], func=AF.Sqrt, R=[ssq], W=[ssq])
            yield
            P.op("dve", "reciprocal", out=ssq[:], in_=ssq[:], R=[ssq], W=[ssq])
            P.op("dve", "tensor_tensor", out=O_[:], in0=O_[:], in1=ssq[:].unsqueeze(2).to_broadcast([128, H, 128]), op=ALU.mult,
                 R=[O_, ssq], W=[O_])
            yield
            P.op("dve", "tensor_tensor", out=O_[:], in0=O_[:], in1=gnb[:].rearrange("p (h d) -> p h d", h=H), op=ALU.mult,
                 R=[O_, gnb], W=[O_])
            if kind != "ml":
                P.op("dve", "tensor_tensor", out=O_[:], in0=O_[:], in1=G_[:].rearrange("p (h d) -> p h d", h=H), op=ALU.mult,
                     R=[O_, G_], W=[O_])
            yield
            P.dma("sp", D["ymix"][rows, cfg["ycol"]:cfg["ycol"] + 512].rearrange("p (h d) -> p h d", h=H), O_[:], R=[O_],
                  W=[P.key("st")])

        loads(0)
        if n_it > 1:
            loads(1)
        loads_post(0)
        _interleave([prep(0)])
        for it in range(n_it):
            if it + 2 < n_it:
                loads(it + 2)
            if it + 1 < n_it:
                loads_post(it + 1)
            _interleave([heads(it), prep(it + 1) if it + 1 < n_it else None])
import math
TWO_PI = 2.0 * math.pi


def hy_dims(L):
    nkt = (L + 1 + 127) // 128
    return L // 128, nkt


def hy_tables(L):
    import ml_dtypes
    N = 2 * L
    ntl, nkt = hy_dims(L)
    n = np.arange(L, dtype=np.int64)[:, None]
    k = np.arange(nkt * 128, dtype=np.int64)[None, :]
    ang = (n * k % N).astype(np.float64) * (TWO_PI / N)
    valid = (k <= L)
    Cf = np.where(valid, np.cos(ang), 0.0)
    Sf = np.where(valid, np.sin(ang), 0.0)
    w = np.where((k == 0) | (k == L), 1.0 / N, 2.0 / N) * valid
    Ci = (Cf * w).T
    Si = (Sf * w).T
    bf = ml_dtypes.bfloat16
    pos = np.arange(L, dtype=np.float32)
    t = pos / np.float32(L - 1)
    fr = np.linspace(1e-4, 15, 16, dtype=np.float32)
    a32 = (np.float32(2.0 * math.pi / L) * pos[:, None] * fr[None, :]).astype(np.float32)
    z = np.concatenate([t[:, None], np.cos(a32), -np.sin(a32)], -1).astype(np.float32)
    tcol = np.ascontiguousarray((-t).reshape(ntl, 128).T).astype(np.float32)
    m0 = np.ones((128, 1), np.float32)
    m0[0, 0] = 0.0
    return {f"k_hyCf{L}": Cf.astype(bf), f"k_hySf{L}": Sf.astype(bf), f"k_hyCi{L}": np.ascontiguousarray(Ci).astype(bf),
            f"k_hySi{L}": np.ascontiguousarray(Si).astype(bf), f"k_hyz{L}": np.ascontiguousarray(z.T),
            f"k_hynt{L}": tcol, "k_m0": m0}


def hy_spec(L):
    ntl, nkt = hy_dims(L)
    return {f"k_hyCf{L}": ((L, nkt * 128), BF16, "ExternalInput"), f"k_hySf{L}": ((L, nkt * 128), BF16, "ExternalInput"),
            f"k_hyCi{L}": ((nkt * 128, L), BF16, "ExternalInput"), f"k_hySi{L}": ((nkt * 128, L), BF16, "ExternalInput"),
            f"k_hyz{L}": ((33, L), F32, "ExternalInput"), f"k_hynt{L}": ((128, ntl), F32, "ExternalInput"),
            "k_m0": ((128, 1), F32, "ExternalInput"),
            f"hyhsd{L}": ((2, L, 1024), BF16, "Internal"), f"hyH{L}": ((2, 2, nkt * 128, 512), F32, "Internal")}


HY_SPEC = {"hyu": ((T_ALL, 1536), F32, "Internal"), "hyz1": ((T_ALL, 512), F32, "Internal")}
HY_SPEC.update(hy_spec(T_LAT))
HY_SPEC.update(hy_spec(T_CTX))


def stage_hy_conv(P, D, l, tiles):
    with P.stage("hy_conv"):
        W = 1536
        wt = [P.sb([128, W]) for _ in range(4)]
        for k in range(3):
            P.dma("sp", wt[k][:], D["hy_conv_w"][l, k].partition_broadcast(128), W=[wt[k]])
        P.dma("sp", wt[3][:], D["hy_conv_b"][l].partition_broadcast(128), W=[wt[3]])
        um = [P.sb([128, W]) for _ in range(2)]
        uc = [P.sb([128, W]) for _ in range(2)]
        up = [P.sb([128, W]) for _ in range(2)]
        acc = [P.sb([128, W]) for _ in range(2)]
        tmp = P.sb([128, W])
        uk = P.key("hyu")
        def loads(i):
            t = tiles[i]
            r0 = t * 128
            a, b, c = um[i % 2], uc[i % 2], up[i % 2]
            first = t in (0, 2)
            last = t in (1, NT - 1)
            P.dma("sp", b[:], D["p"][r0:r0 + 128, 0:W], W=[b])
            if first:
                P.op("dve", "memset", ap=a[:], constant=0.0, W=[a])
                P.dma("sp", a[1:128, :], D["p"][r0:r0 + 127, 0:W], W=[a])
            else:
                P.dma("sp", a[:], D["p"][r0 - 1:r0 + 127, 0:W], W=[a])
            if last:
                P.op("dve", "memset", ap=c[:], constant=0.0, W=[c])
                P.dma("sp", c[0:127, :], D["p"][r0 + 1:r0 + 128, 0:W], W=[c])
            else:
                P.dma("sp", c[:], D["p"][r0 + 1:r0 + 129, 0:W], W=[c])

        loads(0)
        for i, t in enumerate(tiles):
            r0 = t * 128
            a, b, c, o = um[i % 2], uc[i % 2], up[i % 2], acc[i % 2]
            if i + 1 < len(tiles):
                loads(i + 1)
            P.op("dve", "tensor_tensor", out=o[:], in0=a[:], in1=wt[0][:], op=ALU.mult, R=[a, wt[0]], W=[o])
            P.op("dve", "tensor_tensor", out=tmp[:], in0=b[:], in1=wt[1][:], op=ALU.mult, R=[b, wt[1]], W=[tmp])
            P.op("dve", "tensor_tensor", out=o[:], in0=o[:], in1=tmp[:], op=ALU.add, R=[o, tmp], W=[o])
            P.op("dve", "tensor_tensor", out=tmp[:], in0=c[:], in1=wt[2][:], op=ALU.mult, R=[c, wt[2]], W=[tmp])
            P.op("dve", "tensor_tensor", out=o[:], in0=o[:], in1=tmp[:], op=ALU.add, R=[o, tmp], W=[o])
            P.op("dve", "tensor_tensor", out=o[:], in0=o[:], in1=wt[3][:], op=ALU.add, R=[o, wt[3]], W=[o])
            P.dma("sp", D["hyu"][r0:r0 + 128, :], o[:], R=[o], W=[P.key("st")])


def stage_hy_filt(P, D, l, L):
    ntl, nkt = hy_dims(L)
    with P.stage(f"hy_filt{L}"):
        zT = P.sb([33, L])
        P.dma("sp", zT[:], D[f"k_hyz{L}"], W=[zT])
        w1 = P.sb([33, 64])
        w2 = P.sb([64, 64])
        w3 = P.sb([64, 2048])
        P.dma("sp", w1[:], D["hy_f_w1"][l], W=[w1])
        P.dma("sp", w2[:], D["hy_f_w2"][l], W=[w2])
        P.dma("sp", w3[:], D["hy_f_w3"][l], W=[w3])
        cols = P.sb([64, 4])
        P.dma("sp", cols[:, 0:1], D["hy_f_b1"][l].rearrange("(p o) -> p o", o=1), W=[cols])
        P.dma("sp", cols[:, 1:2], D["hy_f_b2"][l].rearrange("(p o) -> p o", o=1), W=[cols])
        P.dma("sp", cols[:, 2:3], D["hy_f_freq"][l, 0].rearrange("(p o) -> p o", o=1), W=[cols])
        P.dma("sp", cols[:, 3:4], D["hy_f_freq"][l, 1].rearrange("(p o) -> p o", o=1), W=[cols])
        ntc = P.sb([128, ntl])
        P.dma("sp", ntc[:], D[f"k_hynt{L}"], W=[ntc])
        m0 = P.sb([128, 1])
        P.dma("sp", m0[:], D["k_m0"], W=[m0])
        dec = P.sb([128, 2048])
        P.dma("sp", dec[:], D["hy_decay"][l].partition_broadcast(128), W=[dec])
        P.op("act", "activation", out=dec[:], in_=dec[:], func=AF.Abs, R=[dec], W=[dec])
        h1 = P.sb([64, L])
        h2 = P.sb([64, L])
        arg = P.sb([64, 512])
        arg2 = P.sb([64, 512])
        pm = [P.ps([64, 512]) for _ in range(2)]
        CW = min(512, L)

        def sin_layer(src_ps, dst, bi, fi, dbuf):
            P.op("dve", "tensor_scalar", out=arg[:, 0:CW], in0=src_ps[:, 0:CW], scalar1=cols[:, bi:bi + 1], scalar2=cols[:, fi:fi + 1],
                 op0=ALU.add, op1=ALU.mult, R=[src_ps, cols], W=[arg])
            P.op("dve", "tensor_scalar", out=arg2[:, 0:CW], in0=arg[:, 0:CW], scalar1=math.pi, scalar2=-TWO_PI, op0=ALU.is_gt, op1=ALU.mult,
                 R=[arg], W=[arg2])
            P.op("dve", "tensor_tensor", out=arg[:, 0:CW], in0=arg[:, 0:CW], in1=arg2[:, 0:CW], op=ALU.add, R=[arg, arg2], W=[arg])
            P.op("dve", "tensor_scalar", out=arg2[:, 0:CW], in0=arg[:, 0:CW], scalar1=-math.pi, scalar2=TWO_PI, op0=ALU.is_lt, op1=ALU.mult,
                 R=[arg], W=[arg2])
            P.op("dve", "tensor_tensor", out=arg[:, 0:CW], in0=arg[:, 0:CW], in1=arg2[:, 0:CW], op=ALU.add, R=[arg, arg2], W=[arg])
            P.op("dve", "tensor_scalar", out=arg[:, 0:CW], in0=arg[:, 0:CW], scalar1=3.141592, scalar2=-3.141592, op0=ALU.min, op1=ALU.max,
                 R=[arg], W=[arg])
            P.op("act", "activation", out=dst, in_=arg[:, 0:CW], func=AF.Sin, R=[arg], W=[dbuf])

        for ci in range(L // CW):
            cs = slice(ci * CW, (ci + 1) * CW)
            pp = pm[ci % 2]
            P.op("pe", "matmul", out=pp[:, 0:CW], lhsT=w1[:], rhs=zT[:, cs], start=True, stop=True, R=[w1, zT], W=[pp])
            sin_layer(pp, h1[:, cs], 0, 2, h1)
        for ci in range(L // CW):
            cs = slice(ci * CW, (ci + 1) * CW)
            pp = pm[ci % 2]
            P.op("pe", "matmul", out=pp[:, 0:CW], lhsT=w2[:], rhs=h1[:, cs], start=True, stop=True, R=[w2, h1], W=[pp])
            sin_layer(pp, h2[:, cs], 1, 3, h2)
        ph = [P.ps([128, 512]) for _ in range(4)]
        env = [P.sb([128, 2048]) for _ in range(2)]
        hh = [P.sb([128, 2048]) for _ in range(2)]
        hs = [P.sb([128, 1024], BF16) for _ in range(2)]
        hd = [P.sb([128, 1024], BF16) for _ in range(2)]
        hk = P.key("hyhsd")
        for tt in range(ntl):
            e_, h_, s_, d_ = env[tt % 2], hh[tt % 2], hs[tt % 2], hd[tt % 2]
            P.op("act", "activation", out=e_[:], in_=dec[:], func=AF.Exp, scale=ntc[:, tt:tt + 1], R=[dec, ntc], W=[e_])
            for q in range(4):
                P.op("pe", "matmul", out=ph[q][:], lhsT=h2[:, tt * 128:(tt + 1) * 128], rhs=w3[:, q * 512:(q + 1) * 512],
                     start=True, stop=True, R=[h2, w3], W=[ph[q]])
                P.op("dve", "tensor_tensor", out=h_[:, q * 512:(q + 1) * 512], in0=ph[q][:], in1=e_[:, q * 512:(q + 1) * 512],
                     op=ALU.mult, R=[ph[q], e_], W=[h_])
            for o in range(2):
                f_ = h_[:, o * 1024:o * 1024 + 512]
                b_ = h_[:, o * 1024 + 512:(o + 1) * 1024]
                if tt == 0:
                    P.op("dve", "tensor_scalar", out=b_, in0=b_, scalar1=m0[:, 0:1], scalar2=None, op0=ALU.mult, R=[h_, m0], W=[h_])
                P.op("dve", "tensor_tensor", out=s_[:, o * 512:(o + 1) * 512], in0=f_, in1=b_, op=ALU.add, R=[h_], W=[s_])
                P.op("dve", "tensor_tensor", out=d_[:, o * 512:(o + 1) * 512], in0=b_, in1=f_, op=ALU.subtract, R=[h_], W=[d_])
            P.dma("sp", D[f"hyhsd{L}"][0, tt * 128:(tt + 1) * 128, :], s_[:], R=[s_], W=[P.key("st")])
            P.dma("sp", D[f"hyhsd{L}"][1, tt * 128:(tt + 1) * 128, :], d_[:], R=[d_], W=[P.key("st")])


def stage_hy_filt_fft(P, D, L):
    ntl, nkt = hy_dims(L)
    with P.stage(f"hy_hfft{L}"):
        HS = P.sb([128, ntl, 1024], BF16)
        HDd = P.sb([128, ntl, 1024], BF16)
        half = max(1, ntl // 2)
        for a in range(0, ntl, half):
            P.dma("sp", HS[:, a:a + half, :], D[f"hyhsd{L}"][0, a * 128:(a + half) * 128, :].rearrange("(nt p) c -> p nt c", p=128), W=[HS])
            P.dma("sp", HDd[:, a:a + half, :], D[f"hyhsd{L}"][1, a * 128:(a + half) * 128, :].rearrange("(nt p) c -> p nt c", p=128), W=[HDd])
        tc_ = [P.sb([128, ntl, 128], BF16) for _ in range(2)]
        ts_ = [P.sb([128, ntl, 128], BF16) for _ in range(2)]
        pp = [P.ps([128, 512]) for _ in range(4)]
        ob = [P.sb([128, 512]) for _ in range(4)]
        Hk = P.key("hyH")
        def tloads(kt):
            P.dma("sp", tc_[kt % 2][:], D[f"k_hyCf{L}"][:, kt * 128:(kt + 1) * 128].rearrange("(nt p) k -> p nt k", p=128), W=[tc_[kt % 2]])
            P.dma("sp", ts_[kt % 2][:], D[f"k_hySf{L}"][:, kt * 128:(kt + 1) * 128].rearrange("(nt p) k -> p nt k", p=128), W=[ts_[kt % 2]])

        tloads(0)
        for kt in range(nkt):
            c_, s_ = tc_[kt % 2], ts_[kt % 2]
            if kt + 1 < nkt:
                tloads(kt + 1)
            for o in range(2):
                for ri, (tab, src) in enumerate(((c_, HS), (s_, HDd))):
                    acc = pp[o * 2 + ri]
                    for nt in range(ntl):
                        P.op("pe", "matmul", out=acc[:], lhsT=tab[:, nt, :], rhs=src[:, nt, o * 512:(o + 1) * 512], start=(nt == 0),
                             stop=(nt == ntl - 1), R=[tab, src], W=[acc], tw=(nt == ntl - 1))
                    o_ = ob[o * 2 + ri]
                    if ri == 0:
                        P.op("act", "copy", out=o_[:], in_=acc[:], R=[acc], W=[o_])
                    else:
                        P.op("dve", "tensor_copy", out=o_[:], in_=acc[:], R=[acc], W=[o_])
                    P.dma("sp", D[f"hyH{L}"][o, ri, kt * 128:(kt + 1) * 128, :], o_[:], R=[o_], W=[P.key("st")])


def stage_hy_lconv(P, D, l, L, o, row0):
    ntl, nkt = hy_dims(L)
    zsrc = D["hyu"][row0:row0 + L, 0:512] if o == 0 else D["hyz1"][row0:row0 + L, :]
    gsrc = D["hyu"][row0:row0 + L, (o + 1) * 512:(o + 2) * 512]
    dst = D["hyz1"][row0:row0 + L, :] if o == 0 else D["ymix"][row0:row0 + L, 0:512]
    with P.stage(f"hy_lconv{L}_{o}"):
        Z = P.sb([128, ntl, 512], BF16)
        half = max(1, ntl // 2)
        for a in range(0, ntl, half):
            P.dma("pool", Z[:, a:a + half, :], zsrc[a * 128:(a + half) * 128, :].rearrange("(nt p) c -> p nt c", p=128), W=[Z])
        Y = P.sb([128, nkt, 2, 512], BF16)
        tc_ = [P.sb([128, ntl, 128], BF16) for _ in range(2)]
        ts_ = [P.sb([128, ntl, 128], BF16) for _ in range(2)]
        pX = [P.ps([128, 512]) for _ in range(4)]
        Xr, Xi = [P.sb([128, 512]) for _ in range(2)], [P.sb([128, 512]) for _ in range(2)]
        Hr, Hi = [P.sb([128, 512]) for _ in range(2)], [P.sb([128, 512]) for _ in range(2)]
        t1, t2, t3, t4 = P.sb([128, 512]), P.sb([128, 512]), P.sb([128, 512]), P.sb([128, 512])
        for kt in range(nkt):
            c_, s_ = tc_[kt % 2], ts_[kt % 2]
            P.dma("sp", c_[:], D[f"k_hyCf{L}"][:, kt * 128:(kt + 1) * 128].rearrange("(nt p) k -> p nt k", p=128), W=[c_])
            P.dma("sp", s_[:], D[f"k_hySf{L}"][:, kt * 128:(kt + 1) * 128].rearrange("(nt p) k -> p nt k", p=128), W=[s_])
            hr, hi, xr, xi = Hr[kt % 2], Hi[kt % 2], Xr[kt % 2], Xi[kt % 2]
            P.dma("sp", hr[:], D[f"hyH{L}"][o, 0, kt * 128:(kt + 1) * 128, :], W=[hr])
            P.dma("sp", hi[:], D[f"hyH{L}"][o, 1, kt * 128:(kt + 1) * 128, :], W=[hi])
            pr, pi = pX[(kt % 2) * 2], pX[(kt % 2) * 2 + 1]
            for nt in range(ntl):
                P.op("pe", "matmul", out=pr[:], lhsT=c_[:, nt, :], rhs=Z[:, nt, :], start=(nt == 0), stop=(nt == ntl - 1),
                     R=[c_, Z], W=[pr], tw=(nt == ntl - 1))
            for nt in range(ntl):
                P.op("pe", "matmul", out=pi[:], lhsT=s_[:, nt, :], rhs=Z[:, nt, :], start=(nt == 0), stop=(nt == ntl - 1),
                     R=[s_, Z], W=[pi], tw=(nt == ntl - 1))
            P.op("act", "copy", out=xr[:], in_=pr[:], R=[pr], W=[xr])
            P.op("act", "copy", out=xi[:], in_=pi[:], R=[pi], W=[xi])
            P.op("dve", "tensor_tensor", out=t1[:], in0=xr[:], in1=hr[:], op=ALU.mult, R=[xr, hr], W=[t1])
            P.op("dve", "tensor_tensor", out=t2[:], in0=xi[:], in1=hi[:], op=ALU.mult, R=[xi, hi], W=[t2])
            P.op("dve", "tensor_tensor", out=Y[:, kt, 0, :], in0=t1[:], in1=t2[:], op=ALU.add, R=[t1, t2], W=[Y])
            P.op("pool", "tensor_tensor", out=t3[:], in0=xi[:], in1=hr[:], op=ALU.mult, R=[xi, hr], W=[t3])
            P.op("pool", "tensor_tensor", out=t4[:], in0=xr[:], in1=hi[:], op=ALU.mult, R=[xr, hi], W=[t4])
            P.op("pool", "tensor_tensor", out=Y[:, kt, 1, :], in0=t3[:], in1=t4[:], op=ALU.subtract, R=[t3, t4], W=[Y])
        ic_ = [P.sb([128, nkt, 128], BF16) for _ in range(2)]
        is_ = [P.sb([128, nkt, 128], BF16) for _ in range(2)]
        skb = P.sb([128, 512])
        P.dma("sp", skb[:], D["hy_skip"][l, o].partition_broadcast(128), W=[skb])
        zt = [P.sb([128, 512]) for _ in range(2)]
        gt = [P.sb([128, 512]) for _ in range(2)]
        ot = [P.sb([128, 512]) for _ in range(2)]
        dk_ = P.key("hydst")
        def iloads(nt):
            P.dma("sp", ic_[nt % 2][:], D[f"k_hyCi{L}"][:, nt * 128:(nt + 1) * 128].rearrange("(kt p) n -> p kt n", p=128), W=[ic_[nt % 2]])
            P.dma("sp", is_[nt % 2][:], D[f"k_hySi{L}"][:, nt * 128:(nt + 1) * 128].rearrange("(kt p) n -> p kt n", p=128), W=[is_[nt % 2]])
            P.dma("sp", zt[nt % 2][:], zsrc[nt * 128:(nt + 1) * 128, :], W=[zt[nt % 2]])
            P.dma("sp", gt[nt % 2][:], gsrc[nt * 128:(nt + 1) * 128, :], W=[gt[nt % 2]])

        iloads(0)
        for nt in range(ntl):
            c_, s_, z_, g_, o_ = ic_[nt % 2], is_[nt % 2], zt[nt % 2], gt[nt % 2], ot[nt % 2]
            if nt + 1 < ntl:
                iloads(nt + 1)
            py = pX[nt % 2]
            for kt in range(nkt):
                P.op("pe", "matmul", out=py[:], lhsT=c_[:, kt, :], rhs=Y[:, kt, 0, :], start=(kt == 0), stop=False,
                     R=[c_, Y], W=[py], tw=False)
            for kt in range(nkt):
                P.op("pe", "matmul", out=py[:], lhsT=s_[:, kt, :], rhs=Y[:, kt, 1, :], start=False, stop=(kt == nkt - 1),
                     R=[s_, Y], W=[py], tw=(kt == nkt - 1))
            P.op("pool", "tensor_tensor", out=z_[:], in0=z_[:], in1=skb[:], op=ALU.mult, R=[z_, skb], W=[z_])
            P.op("dve", "tensor_tensor", out=o_[:], in0=py[:], in1=z_[:], op=ALU.add, R=[py, z_], W=[o_])
            P.op("dve", "tensor_tensor", out=o_[:], in0=o_[:], in1=g_[:], op=ALU.mult, R=[o_, g_], W=[o_])
            P.dma("sp", dst[nt * 128:(nt + 1) * 128, :], o_[:], R=[o_], W=[P.key("st")])


def stage_hyena(P, D, l, with_ctx):
    tiles = list(range(NT)) if with_ctx else list(range(2, NT))
    stage_hy_conv(P, D, l, tiles)
    seqs = ([(T_CTX, 0)] if with_ctx else []) + [(T_LAT, T_CTX)]
    for (L, row0) in seqs:
        stage_hy_filt(P, D, l, L)
        stage_hy_filt_fft(P, D, L)
        for o in range(2):
            stage_hy_lconv(P, D, l, L, o, row0)
SPEC = {
    "x": ((T_LAT, DM), F32, "ExternalInput"), "ctx": ((T_CTX, DM), F32, "ExternalInput"),
    "c": ((DM,), F32, "ExternalInput"), "c_ctx": ((DM,), F32, "ExternalInput"),
    "ada_w": ((DEPTH, DM, 6 * DM), F32, "ExternalInput"), "ada_b": ((DEPTH, 6 * DM), F32, "ExternalInput"),
    "norm1_g": ((DEPTH, DM), F32, "ExternalInput"), "norm2_g": ((DEPTH, DM), F32, "ExternalInput"),
    "w_in": ((DEPTH, DM, DPROJ), F32, "ExternalInput"),
    "hy_conv_w": ((DEPTH, 3, 1536), F32, "ExternalInput"), "hy_conv_b": ((DEPTH, 1536), F32, "ExternalInput"),
    "hy_f_w1": ((DEPTH, 33, 64), F32, "ExternalInput"), "hy_f_b1": ((DEPTH, 64), F32, "ExternalInput"),
    "hy_f_w2": ((DEPTH, 64, 64), F32, "ExternalInput"), "hy_f_b2": ((DEPTH, 64), F32, "ExternalInput"),
    "hy_f_freq": ((DEPTH, 2, 64), F32, "ExternalInput"), "hy_f_w3": ((DEPTH, 64, 2048), F32, "ExternalInput"),
    "hy_decay": ((DEPTH, 2048), F32, "ExternalInput"), "hy_skip": ((DEPTH, 2, 512), F32, "ExternalInput"),
    "gla_wa2": ((DEPTH, 2, 16, 256), F32, "ExternalInput"), "gla_ba": ((DEPTH, 2, 256), F32, "ExternalInput"),
    "gla_norm_g": ((DEPTH, 512), F32, "ExternalInput"), "ml_gate_b": ((DEPTH, 2, 2, 4), F32, "ExternalInput"),
    "ml_norm_g": ((DEPTH, 512), F32, "ExternalInput"), "ret_decay": ((DEPTH, 2, 4), F32, "ExternalInput"),
    "ret_norm_g": ((DEPTH, 512), F32, "ExternalInput"), "w_out": ((DEPTH, DM, DM), F32, "ExternalInput"),
    "moe_wg": ((DEPTH, DM, 4), F32, "ExternalInput"), "moe_bg": ((DEPTH, 4), F32, "ExternalInput"),
    "moe_we": ((DEPTH, DM, 32), F32, "ExternalInput"), "moe_be": ((DEPTH, 32), F32, "ExternalInput"),
    "moe_w1": ((DEPTH, 4, 8, DM, FF), F32, "ExternalInput"), "moe_w3": ((DEPTH, 4, 8, DM, FF), F32, "ExternalInput"),
    "moe_w2": ((DEPTH, 4, 8, FF, DM), F32, "ExternalInput"), "final_g": ((DM,), F32, "ExternalInput"),
    "k_ident": ((128, 128), F32, "ExternalInput"), "k_tri": ((3, 128, 128), F32, "ExternalInput"),
    "k_sel": ((32, 32, 128), F32, "ExternalInput"),
    "k_cos": ((T_LAT, 64), F32, "ExternalInput"), "k_sin": ((T_LAT, 64), F32, "ExternalInput"),
    "res": ((T_ALL, DM), F32, "Internal"), "modd": ((DEPTH, 2, 6 * DM), F32, "Internal"),
    "p": ((T_ALL, DPROJ), F32, "Internal"), "ymix": ((T_ALL, DM), F32, "Internal"),
    "xn2T": ((128, KC, T_ALL), BF16, "Internal"), "combT": ((32, T_ALL), F32, "Internal"),
    "oacc": ((T_ALL, 512), F32, "Internal"),
    "out": ((T_HALF, DM), F32, "ExternalOutput"),
    "res_h": ((T_HALF, DM), F32, "Internal"),
    "k_rows": ((128, T_HALF // 128), I32, "ExternalInput"),
}


SPEC.update(HY_SPEC)


class DramTable(dict):
    def __init__(self, nc, ext=None):
        super().__init__()
        self.nc, self.ext = nc, dict(ext or {})

    def __missing__(self, name):
        shape, dt, kind = SPEC[name]
        kind = self.ext.get(name, kind)
        ap = self.nc.dram_tensor(name, list(shape), dt, kind=kind).ap()
        self[name] = ap
        return ap


def host_consts():
    j = np.arange(128)[:, None]
    i = np.arange(128)[None, :]
    tri = np.stack([(j <= i), (j >= i), np.ones((128, 128), bool)]).astype(np.float32)
    sel = np.zeros((32, 32, 128), np.float32)
    for e in range(32):
        sel[e, e, :] = 1.0
    tt = np.arange(T_LAT)
    inv = (10000.0 ** (-np.arange(32, dtype=np.float32) / np.float32(32))).astype(np.float32)
    ang = np.concatenate([(tt // 64).astype(np.float32)[:, None] * inv, (tt % 64).astype(np.float32)[:, None] * inv], -1)
    out = {"k_ident": np.eye(128, dtype=np.float32), "k_tri": tri, "k_sel": sel,
           "k_cos": np.cos(ang).astype(np.float32), "k_sin": np.sin(ang).astype(np.float32)}
    out.update(hy_tables(T_LAT))
    out.update(hy_tables(T_CTX))
    return out


def half_rows(h):
    r = T_CTX + h * T_HALF + np.arange(T_HALF, dtype=np.int32)
    return np.ascontiguousarray(r.reshape(T_HALF // 128, 128).T)


def stage_consts(P, D):
    C = {}
    C["idf"] = P.sb([128, 128], F32, name="c_idf")
    C["idb"] = P.sb([128, 128], BF16, name="c_idb")
    with P.stage("consts"):
        P.dma("sp", C["idf"][:], D["k_ident"], W=[C["idf"]])
        P.op("dve", "tensor_copy", out=C["idb"][:], in_=C["idf"][:], R=[C["idf"]], W=[C["idb"]])
    return C


def build_program():
    nc = bass.Bass("TRN2", target_bir_lowering=False)
    P = Prog(nc)
    D = DramTable(nc)
    C = stage_consts(P, D)
    stage_init(P, D)
    lat_tiles = list(range(2, NT))
    lat_groups = [lat_tiles[i:i + 8] for i in range(0, len(lat_tiles), 8)]
    for l in range(DEPTH):
        with_ctx = l < DEPTH - 1
        stage_mod(P, D, l)
        stage_inproj(P, D, l, C)
        stage_hyena(P, D, l, with_ctx)
        for kind in ("gla", "ml", "ret"):
            for dr in range(2):
                stage_linattn(P, D, l, C, kind, dr, with_ctx)
        if with_ctx:
            tiles = list(range(NT))
            stage_outproj(P, D, l, C, tiles)
            stage_moe_a(P, D, l, C, tiles)
            stage_moe_b(P, D, l, C, [[0, 1]] + lat_groups)
        else:
            ht = list(range(T_HALF // 128))
            stage_outproj(P, D, l, C, ht, gather=True)
            stage_moe_a(P, D, l, C, ht, res="res_h", lat_only=True)
            stage_moe_b(P, D, l, C, [ht[i:i + 8] for i in range(0, len(ht), 8)], res="res_h", lat_only=True)
    stage_final(P, D)
    P.finish()
    P.close()
    in_names = [n for n in D if SPEC[n][2] == "ExternalInput"]
    return nc, in_names


_CACHE = {}


def kernel(**inputs):
    if "prog" not in _CACHE:
        _CACHE["prog"] = build_program()
        _CACHE["consts"] = host_consts()
    nc, in_names = _CACHE["prog"]
    consts = _CACHE["consts"]
    n_cores = 8
    shared = {}
    for n in in_names:
        if n in consts:
            shared[n] = consts[n]
        elif n not in ("x", "ctx", "c", "k_rows"):
            shared[n] = np.ascontiguousarray(np.asarray(inputs[n], dtype=np.float32))
    in_maps = []
    for i in range(n_cores):
        b = i // 2
        m = dict(shared)
        for n in ("x", "ctx", "c"):
            m[n] = np.ascontiguousarray(np.asarray(inputs[n][b], dtype=np.float32))
        m["k_rows"] = half_rows(i % 2)
        in_maps.append(m)
    res = run_bass_kernel_spmd(nc, in_maps, core_ids=list(range(n_cores)))
    out = np.stack([np.concatenate([np.asarray(res.results[2 * b + h]["out"]) for h in range(2)], axis=0)
                    for b in range(4)], axis=0)
    return out.astype(np.float32)
```

```python
import contextlib
import numpy as np
import concourse.bass as bass
import concourse.mybir as mybir

F32 = mybir.dt.float32
BF16 = mybir.dt.bfloat16
I32 = mybir.dt.int32
AF = mybir.ActivationFunctionType
ALU = mybir.AluOpType
AX = mybir.AxisListType

ENGS = ("pe", "act", "dve", "pool", "sp")
N_DMA_SEM = 8
N_C_SEM = 4


class Buf:
    __slots__ = ("name", "t", "last_w", "reads", "psum")

    def __init__(self, name, t=None, psum=False):
        self.name = name
        self.t = t
        self.psum = psum
        self.last_w = None
        self.reads = []

    def __getitem__(self, idx):
        return self.t[idx]


class Op:
    __slots__ = ("eng", "fn", "waits", "ev", "is_dma", "needed", "stage")


class Prog:
    def __init__(self, nc):
        self.nc = nc
        self.stack = contextlib.ExitStack()
        self.base = self.stack
        self.sems = None
        self.sigval = {(e, r): 0 for e in ENGS for r in range(N_C_SEM)}
        self.barrier = []
        self.val = {}
        self.stage_id = 0
        self.q = {e: [] for e in ENGS}
        self.seen = {e: {} for e in ENGS}
        self.sig_count = {e: 0 for e in ENGS}
        self.dma_count = {e: 0 for e in ENGS}
        self.dma_events = {e: [] for e in ENGS}
        self.nbuf = 0

    def sb(self, shape, dt=F32, name=None):
        self.nbuf += 1
        name = name or f"sb{self.nbuf}"
        t = self.stack.enter_context(self.nc.sbuf_tensor(name, list(shape), dt))
        return Buf(name, t)

    def ps(self, shape, dt=F32, name=None):
        self.nbuf += 1
        name = name or f"ps{self.nbuf}"
        per_bank = 512 if dt == F32 else 1024
        full = self.stack.enter_context(self.nc.psum_tensor(name, [128, per_bank], dt))
        shape = list(shape)
        n = 1
        for d in shape[1:]:
            n *= d
        assert n <= per_bank and shape[0] <= 128
        v = full[0:shape[0], 0:n]
        if len(shape) == 3:
            v = v.rearrange("p (a b) -> p a b", a=shape[1])
        return Buf(name, v, psum=True)

    def dram(self, name, shape, dt=F32, kind="Internal"):
        h = self.nc.dram_tensor(name, list(shape), dt, kind=kind)
        return h.ap()

    def key(self, name):
        return Buf(name)

    def _deps(self, eng, reads, writes):
        evs = []
        for b in reads:
            if b.last_w is not None:
                evs.append(b.last_w)
            if b.psum:
                evs.extend(ev for ev in b.reads if ev[2].eng != eng)
        for b in writes:
            if b.last_w is not None:
                evs.append(b.last_w)
            evs.extend(b.reads)
        return evs

    def _mk_waits(self, eng, evs):
        need = {}
        for (k, v, op) in evs:
            if op.stage != self.stage_id or self.seen[eng].get(k, 0) >= v:
                continue
            if need.get(k, (0, None))[0] < v:
                need[k] = (v, op)
        waits = []
        for k, (v, op) in need.items():
            self.seen[eng][k] = v
            op.needed = True
            waits.append((k, op))
        return waits

    def op(self, eng, name, R=(), W=(), tw=True, **kw):
        reads, writes, track_write = R, W, tw
        fn = (lambda e, name=name, kw=kw: getattr(e, name)(**kw))
        o = Op()
        o.stage = self.stage_id
        o.eng, o.fn, o.is_dma, o.needed = eng, fn, False, False
        o.waits = self._mk_waits(eng, self._deps(eng, reads, writes))
        i = self.sig_count[eng]
        self.sig_count[eng] += 1
        o.ev = (("c", eng, i % N_C_SEM), i // N_C_SEM + 1, o)
        self.q[eng].append(o)
        for b in reads:
            b.reads.append(o.ev)
        if track_write:
            for b in writes:
                b.last_w = o.ev
                b.reads = []
        return o

    def dma(self, eng, out, in_, R=(), W=(), **kw):
        reads, writes = R, W
        o = Op()
        o.stage = self.stage_id
        o.eng, o.is_dma, o.needed = eng, True, True
        i = self.dma_count[eng]
        self.dma_count[eng] += 1
        r = i % N_DMA_SEM
        evs = self._deps(eng, reads, writes)
        if i >= N_DMA_SEM:
            evs.append(self.dma_events[eng][i - N_DMA_SEM])
        o.waits = self._mk_waits(eng, evs)
        o.ev = (("d", eng, r), 16 * (i // N_DMA_SEM + 1), o)
        self.dma_events[eng].append(o.ev)
        custom = kw.pop("custom", None)
        if custom is not None:
            o.fn = custom
        else:
            o.fn = lambda e, out=out, in_=in_, kw=kw: e.dma_start(out=out, in_=in_, **kw)
        self.q[eng].append(o)
        for b in reads:
            b.reads.append(o.ev)
        for b in writes:
            b.last_w = o.ev
            b.reads = []
        return o

    @contextlib.contextmanager
    def stage(self, name=""):
        sub = contextlib.ExitStack()
        self.stack = sub
        self.nstage = getattr(self, "nstage", 0) + 1
        try:
            yield
            with self.nc.named_scope(f"s{self.nstage:02d}_{name}"):
                self.emit()
        finally:
            self.stack = self.base
            sub.close()

    def emit(self):
        nc = self.nc
        if self.sems is None:
            self.sems = {}
            for e in ENGS:
                for r in range(N_C_SEM):
                    self.sems[("c", e, r)] = self.base.enter_context(nc.semaphore(f"c_{e}{r}"))
                for r in range(N_DMA_SEM):
                    self.sems[("d", e, r)] = self.base.enter_context(nc.semaphore(f"d_{e}{r}"))
        sems = self.sems
        nxt = []
        for e in ENGS:
            comp = [o for o in self.q[e] if not o.is_dma and o.ev[1] > 0]
            if comp:
                comp[-1].needed = True
                nxt.append(comp[-1].ev)
            last = {}
            for o in self.q[e]:
                if o.is_dma:
                    last[o.ev[0]] = o.ev
            nxt.extend(last.values())
        val = self.val
        for e in ENGS:
            for o in self.q[e]:
                if o.is_dma:
                    val[id(o)] = o.ev[1]
                elif o.needed:
                    kk = (e, o.ev[0][2])
                    self.sigval[kk] += 1
                    val[id(o)] = self.sigval[kk]
        prologue = {}
        for e in ENGS:
            w = []
            for (k, v, dep) in self.barrier:
                w.append((k, val[id(dep)]))
            prologue[e] = w
        with nc.Block() as block:
            self._emit_block(block, sems, val, prologue)
        self.barrier = nxt
        self.q = {e: [] for e in ENGS}
        self.stage_id += 1

    def _emit_block(self, block, sems, val, prologue):
        getters = {"pe": block.tensor, "act": block.scalar, "dve": block.vector,
                   "pool": block.gpsimd, "sp": block.sync}

        def body(e):
            def f(engine):
                for (k, v) in prologue[e]:
                    engine.wait_ge(sems[k], v)
                for o in self.q[e]:
                    for (k, dep) in o.waits:
                        engine.wait_ge(sems[k], val[id(dep)])
                    ins = o.fn(engine)
                    if o.is_dma:
                        ins.then_inc(sems[o.ev[0]], 16)
                    elif o.needed:
                        ins.then_inc(sems[o.ev[0]], 1)
            return f

        for e in ENGS:
            getters[e](body(e))

    def finish(self):
        with self.stage("final"):
            pass

    def close(self):
        self.stack.close()
from concourse.bass_utils import run_bass_kernel_spmd
T_CTX, T_LAT = 256, 4096
T_ALL = T_CTX + T_LAT
NT = T_ALL // 128
DM = 2048
KC = DM // 128
DPROJ = 7216
EPS = 1e-6
DEPTH = 2
N_EXP, FF = 32, 256
T_HALF = T_LAT // 2

OFF = {}
_o = 0
for _n, _w in (("hy", 1536), ("gla_q", 256), ("gla_k", 256), ("gla_v", 512), ("gla_g", 512), ("gla_r", 32),
               ("ml_q", 512), ("ml_k", 512), ("ml_v", 512), ("ml_o", 512), ("ml_gates", 16),
               ("ret_q", 512), ("ret_k", 512), ("ret_v", 512), ("ret_g", 512)):
    OFF[_n] = _o
    _o += _w
assert _o == DPROJ


def tile_r(t):
    return 1 if t < 2 else 0


def rms_rstd(P, xt, junk, ss, rs, width):
    P.op("act", "activation", out=junk[:], in_=xt[:], func=AF.Square, accum_out=ss[:], R=[xt], W=[junk, ss])
    P.op("dve", "tensor_scalar", out=rs[:], in0=ss[:], scalar1=1.0 / width, scalar2=EPS, op0=ALU.mult, op1=ALU.add,
         R=[ss], W=[rs])
    P.op("act", "activation", out=rs[:], in_=rs[:], func=AF.Sqrt, R=[rs], W=[rs])
    P.op("dve", "reciprocal", out=rs[:], in_=rs[:], R=[rs], W=[rs])


def load_mod_tiles(P, D, l, which, rows=(0, 1), gain=None):
    out = {}
    gb = None
    if gain is not None:
        gb = P.sb([128, DM])
        P.dma("sp", gb[:], gain.partition_broadcast(128), W=[gb])
    for r in rows:
        for ch in which:
            t = P.sb([128, DM])
            P.dma("sp", t[:], D["modd"][l, r, ch * DM:(ch + 1) * DM].partition_broadcast(128), W=[t])
            if ch in (1, 4) and gb is not None:
                P.op("dve", "scalar_tensor_tensor", out=t[:], in0=t[:], scalar=1.0, in1=gb[:], op0=ALU.add, op1=ALU.mult,
                     R=[t, gb], W=[t])
            out[(r, ch)] = t
    return out


def stage_init(P, D):
    with P.stage("init"):
        k = P.key("res_init")
        P.dma("sp", D["res"][0:T_CTX, :], D["ctx"], W=[P.key("st")])
        for i in range(4):
            P.dma("sp", D["res"][T_CTX + i * 1024:T_CTX + (i + 1) * 1024, :], D["x"][i * 1024:(i + 1) * 1024, :], W=[P.key("st")])


def stage_mod(P, D, l):
    with P.stage("mod"):
        cT = P.sb([128, KC, 2])
        sT = P.sb([128, KC, 2])
        P.dma("sp", cT[:, :, 0], D["c"].rearrange("(kc p) -> p kc", p=128), W=[cT], allow_slow_non_contiguous=True)
        P.dma("sp", cT[:, :, 1], D["c_ctx"].rearrange("(kc p) -> p kc", p=128), W=[cT], allow_slow_non_contiguous=True)
        P.op("act", "activation", out=sT[:], in_=cT[:], func=AF.Silu, R=[cT], W=[sT])
        badd = P.sb([2, 6 * DM])
        osb = P.sb([2, 6 * DM])
        for r in range(2):
            P.dma("sp", badd[r:r + 1, :], D["ada_b"][l:l + 1, :], W=[badd])
        wb = [P.sb([128, KC, 512]) for _ in range(2)]
        pm = [P.ps([2, 512]) for _ in range(2)]
        for ng in range(24):
            w = wb[ng % 2]
            P.dma("sp", w[:], D["ada_w"][l, :, ng * 512:(ng + 1) * 512].rearrange("(kc p) n -> p kc n", p=128), W=[w])
            pp = pm[ng % 2]
            for kc in range(KC):
                P.op("pe", "matmul", out=pp[:], lhsT=sT[:, kc, :], rhs=w[:, kc, :], start=(kc == 0), stop=(kc == KC - 1),
                     R=[sT, w], W=[pp], tw=(kc == KC - 1))
            P.op("dve", "tensor_tensor", out=osb[:, ng * 512:(ng + 1) * 512], in0=pp[:], in1=badd[:, ng * 512:(ng + 1) * 512],
                 op=ALU.add, R=[pp, badd], W=[osb])
        P.dma("sp", D["modd"][l], osb[:], R=[osb], W=[P.key("modd")])


def stage_inproj(P, D, l, C):
    with P.stage("inproj"):
        mods = load_mod_tiles(P, D, l, which=(0, 1), gain=D["norm1_g"][l])
        idb = C["idb"]
        SG = 17
        xnT = P.sb([128, KC, SG * 128], BF16)
        xt = [P.sb([128, DM]) for _ in range(2)]
        junk = P.sb([128, DM])
        xn = [P.sb([128, DM], BF16) for _ in range(2)]
        ss = [P.sb([128, 1]) for _ in range(2)]
        rs = [P.sb([128, 1]) for _ in range(2)]
        pt = [P.ps([128, 4, 128], BF16) for _ in range(2)]
        wch = [P.sb([128, KC, 512], BF16) for _ in range(2)]
        po = [P.ps([128, 512]) for _ in range(3)]
        osb = [P.sb([128, 512]) for _ in range(3)]
        pk = P.key("p")
        ncg = (DPROJ + 511) // 512
        for sg in range(NT // SG):
            for ti in range(SG):
                t = sg * SG + ti
                r = tile_r(t)
                x_, xn_, ss_, rs_ = xt[t % 2], xn[t % 2], ss[t % 2], rs[t % 2]
                P.dma("sp", x_[:], D["res"][t * 128:(t + 1) * 128, :], W=[x_])
                rms_rstd(P, x_, junk, ss_, rs_, DM)
                P.op("dve", "scalar_tensor_tensor", out=junk[:], in0=x_[:], scalar=rs_[:, 0:1], in1=mods[(r, 1)][:],
                     op0=ALU.mult, op1=ALU.mult, R=[x_, rs_, mods[(r, 1)]], W=[junk])
                P.op("dve", "tensor_tensor", out=xn_[:], in0=junk[:], in1=mods[(r, 0)][:], op=ALU.add,
                     R=[junk, mods[(r, 0)]], W=[xn_])
                for g in range(4):
                    pp = pt[g % 2]
                    for j in range(4):
                        kc = g * 4 + j
                        P.op("pe", "transpose", out=pp[:, j, :], in_=xn_[:, kc * 128:(kc + 1) * 128], identity=idb[:],
                             R=[xn_, idb], W=[pp])
                    P.op("act", "copy", out=xnT[:, g * 4:(g + 1) * 4, ti * 128:(ti + 1) * 128], in_=pp[:], R=[pp], W=[xnT])
            cnt = 0
            for cg in range(ncg):
                c0 = cg * 512
                cw = min(512, DPROJ - c0)
                w = wch[cg % 2]
                P.dma("pool", w[:, :, 0:cw], D["w_in"][l, :, c0:c0 + cw].rearrange("(kc p) n -> p kc n", p=128), W=[w])
                for ti in range(SG):
                    t = sg * SG + ti
                    pp, ob = po[cnt % 3], osb[cnt % 3]
                    for kc in range(KC):
                        P.op("pe", "matmul", out=pp[:, 0:cw], lhsT=xnT[:, kc, ti * 128:(ti + 1) * 128], rhs=w[:, kc, 0:cw],
                             start=(kc == 0), stop=(kc == KC - 1), R=[xnT, w], W=[pp], tw=(kc == KC - 1))
                    if cnt % 2 == 0:
                        P.op("act", "copy", out=ob[:, 0:cw], in_=pp[:, 0:cw], R=[pp], W=[ob])
                    else:
                        P.op("dve", "tensor_copy", out=ob[:, 0:cw], in_=pp[:, 0:cw], R=[pp], W=[ob])
                    P.dma("sp", D["p"][t * 128:(t + 1) * 128, c0:c0 + cw], ob[:, 0:cw], R=[ob], W=[P.key("st")])
                    cnt += 1


def stage_outproj(P, D, l, C, tiles, gather=False):
    with P.stage("outproj"):
        rows = [0] if gather else sorted(set(tile_r(t) for t in tiles))
        if gather:
            idx = P.sb([128, len(tiles)], I32)
            P.dma("sp", idx[:], D["k_rows"], W=[idx])
        g1 = load_mod_tiles(P, D, l, which=(2,), rows=rows)
        idb = C["idb"]
        wo = P.sb([128, KC, DM], BF16)
        for h in range(4):
            P.dma("pool", wo[:, :, h * 512:(h + 1) * 512],
                  D["w_out"][l, :, h * 512:(h + 1) * 512].rearrange("(kc p) n -> p kc n", p=128), W=[wo])
        yt = [P.sb([128, DM]) for _ in range(2)]
        yb = [P.sb([128, DM], BF16) for _ in range(2)]
        yT = [P.sb([128, KC, 128], BF16) for _ in range(2)]
        xr = [P.sb([128, DM]) for _ in range(2)]
        tmp = [P.sb([128, 512]) for _ in range(2)]
        pt = [P.ps([128, 4, 128], BF16) for _ in range(2)]
        po = [P.ps([128, 512]) for _ in range(2)]
        rk = P.key("res")
        def loads(i):
            t = tiles[i]
            if gather:
                off = bass.IndirectOffsetOnAxis(ap=idx[:, t:t + 1], axis=0)
                for buf, src in ((yt[i % 2], "ymix"), (xr[i % 2], "res")):
                    P.dma("pool", None, None, R=[idx], W=[buf],
                          custom=lambda e, buf=buf, src=src, off=off: e.indirect_dma_start(
                              out=buf[:], out_offset=None, in_=D[src][:, :], in_offset=off))
                return
            P.dma("sp", yt[i % 2][:], D["ymix"][t * 128:(t + 1) * 128, :], W=[yt[i % 2]])
            P.dma("sp", xr[i % 2][:], D["res"][t * 128:(t + 1) * 128, :], W=[xr[i % 2]])

        loads(0)
        for i, t in enumerate(tiles):
            r = 0 if gather else tile_r(t)
            y_, yb_, yT_, x_ = yt[i % 2], yb[i % 2], yT[i % 2], xr[i % 2]
            if i + 1 < len(tiles):
                loads(i + 1)
            P.op("act", "copy", out=yb_[:], in_=y_[:], R=[y_], W=[yb_])
            for g in range(4):
                pp = pt[g % 2]
                for j in range(4):
                    kc = g * 4 + j
                    P.op("pe", "transpose", out=pp[:, j, :], in_=yb_[:, kc * 128:(kc + 1) * 128], identity=idb[:],
                         R=[yb_, idb], W=[pp])
                P.op("act", "copy", out=yT_[:, g * 4:(g + 1) * 4, :], in_=pp[:], R=[pp], W=[yT_])
            for cg in range(4):
                pp, tm = po[cg % 2], tmp[cg % 2]
                for kc in range(KC):
                    P.op("pe", "matmul", out=pp[:], lhsT=yT_[:, kc, :], rhs=wo[:, kc, cg * 512:(cg + 1) * 512],
                         start=(kc == 0), stop=(kc == KC - 1), R=[yT_, wo], W=[pp], tw=(kc == KC - 1))
                P.op("dve", "tensor_tensor", out=tm[:], in0=pp[:], in1=g1[(r, 2)][:, cg * 512:(cg + 1) * 512], op=ALU.mult,
                     R=[pp, g1[(r, 2)]], W=[tm])
                P.op("pool", "tensor_tensor", out=x_[:, cg * 512:(cg + 1) * 512], in0=x_[:, cg * 512:(cg + 1) * 512], in1=tm[:],
                     op=ALU.add, R=[x_, tm], W=[x_])
            P.dma("sp", D["res_h" if gather else "res"][t * 128:(t + 1) * 128, :], x_[:], R=[x_], W=[P.key("st")])


def stage_moe_a(P, D, l, C, tiles, res="res", lat_only=False):
    with P.stage("moe_a"):
        rows = [0] if lat_only else sorted(set(tile_r(t) for t in tiles))
        mods = load_mod_tiles(P, D, l, which=(3, 4), rows=rows, gain=D["norm2_g"][l])
        idf = C["idf"]
        wr = P.sb([128, KC, 36])
        P.dma("sp", wr[:, :, 0:4], D["moe_wg"][l].rearrange("(kc p) n -> p kc n", p=128), W=[wr])
        P.dma("sp", wr[:, :, 4:36], D["moe_we"][l].rearrange("(kc p) n -> p kc n", p=128), W=[wr])
        br = P.sb([128, 36])
        P.dma("sp", br[:, 0:4], D["moe_bg"][l].partition_broadcast(128), W=[br])
        P.dma("sp", br[:, 4:36], D["moe_be"][l].partition_broadcast(128), W=[br])
        xt = [P.sb([128, DM]) for _ in range(2)]
        junk = P.sb([128, DM])
        xn = [P.sb([128, DM]) for _ in range(2)]
        ss = [P.sb([128, 1]) for _ in range(2)]
        rs = [P.sb([128, 1]) for _ in range(2)]
        xTf = [P.sb([128, KC, 128]) for _ in range(2)]
        xTb = [P.sb([128, KC, 128], BF16) for _ in range(2)]
        pt = [P.ps([128, 4, 128]) for _ in range(2)]
        pl = P.ps([128, 36])
        pc = P.ps([32, 128])
        lgs = [P.sb([128, 36]) for _ in range(2)]
        sm = {n: P.sb([128, w]) for n, w in (("gmax", 1), ("ngmax", 1), ("gsum", 1), ("pg", 1), ("gexp", 4), ("gmask", 4),
                                              ("gpen", 4), ("el", 32), ("m1", 1), ("k1", 32), ("el2", 32), ("m2", 1),
                                              ("k2", 32), ("d", 1), ("e", 1), ("w1", 1), ("w2", 1), ("cmb", 32), ("cmb2", 32))}
        cT = [P.sb([32, 128]) for _ in range(2)]
        kx, kc_ = P.key("xn2T"), P.key("combT")
        BIG = 1.0e30
        P.dma("sp", xt[0][:], D[res][tiles[0] * 128:(tiles[0] + 1) * 128, :], W=[xt[0]])
        def part_a(i):
            t = tiles[i]
            r = 0 if lat_only else tile_r(t)
            x_, xn_, ss_, rs_, xTf_, xTb_ = xt[i % 2], xn[i % 2], ss[i % 2], rs[i % 2], xTf[i % 2], xTb[i % 2]
            if i + 1 < len(tiles):
                tn = tiles[i + 1]
                P.dma("sp", xt[(i + 1) % 2][:], D[res][tn * 128:(tn + 1) * 128, :], W=[xt[(i + 1) % 2]])
            rms_rstd(P, x_, junk, ss_, rs_, DM)
            yield
            P.op("dve", "scalar_tensor_tensor", out=junk[:], in0=x_[:], scalar=rs_[:, 0:1], in1=mods[(r, 4)][:],
                 op0=ALU.mult, op1=ALU.mult, R=[x_, rs_, mods[(r, 4)]], W=[junk])
            yield
            P.op("pool", "tensor_tensor", out=xn_[:], in0=junk[:], in1=mods[(r, 3)][:], op=ALU.add,
                 R=[junk, mods[(r, 3)]], W=[xn_])
            yield
            for g in range(4):
                pp = pt[g % 2]
                for j in range(4):
                    kc = g * 4 + j
                    P.op("pe", "transpose", out=pp[:, j, :], in_=xn_[:, kc * 128:(kc + 1) * 128], identity=idf[:],
                         R=[xn_, idf], W=[pp])
                    yield
                P.op("act", "copy", out=xTf_[:, g * 4:(g + 1) * 4, :], in_=pp[:], R=[pp], W=[xTf_])
                yield
            P.op("dve", "tensor_copy", out=xTb_[:], in_=xTf_[:], R=[xTf_], W=[xTb_])
            yield
            P.dma("sp", D["xn2T"][:, :, t * 128:(t + 1) * 128], xTb_[:], R=[xTb_], W=[P.key("st")])
            for kc in range(KC):
                P.op("pe", "matmul", out=pl[:], lhsT=xTf_[:, kc, :], rhs=wr[:, kc, :], start=(kc == 0), stop=(kc == KC - 1),
                     R=[xTf_, wr], W=[pl], tw=(kc == KC - 1))
                yield
            P.op("dve", "tensor_tensor", out=lgs[i % 2][:], in0=pl[:], in1=br[:], op=ALU.add, R=[pl, br], W=[lgs[i % 2]])
            yield
            yield

        def part_b(i):
            t = tiles[i]
            lg = lgs[i % 2]
            s = sm
            V = lambda name, **kw: P.op("dve", name, **kw)
            V("reduce_max", out=s["gmax"][:], in_=lg[:, 0:4], axis=AX.X, R=[lg], W=[s["gmax"]])
            yield
            V("tensor_scalar", out=s["ngmax"][:], in0=s["gmax"][:], scalar1=-1.0, scalar2=None, op0=ALU.mult,
              R=[s["gmax"]], W=[s["ngmax"]])
            yield
            P.op("act", "activation", out=s["gexp"][:], in_=lg[:, 0:4], func=AF.Exp, bias=s["ngmax"][:, 0:1],
                 accum_out=s["gsum"][:], R=[lg, s["ngmax"]], W=[s["gexp"], s["gsum"]])
            yield
            V("reciprocal", out=s["pg"][:], in_=s["gsum"][:], R=[s["gsum"]], W=[s["pg"]])
            yield
            V("tensor_scalar", out=s["gmask"][:], in0=lg[:, 0:4], scalar1=s["gmax"][:, 0:1], scalar2=None, op0=ALU.is_ge,
              R=[lg, s["gmax"]], W=[s["gmask"]])
            yield
            V("tensor_scalar", out=s["gpen"][:], in0=s["gmask"][:], scalar1=-1.0, scalar2=BIG, op0=ALU.add, op1=ALU.mult,
              R=[s["gmask"]], W=[s["gpen"]])
            yield
            V("tensor_tensor", out=s["el"][:].rearrange("p (g e) -> p g e", g=4),
              in0=lg[:, 4:36].rearrange("p (g e) -> p g e", g=4),
              in1=s["gpen"][:].unsqueeze(2).to_broadcast([128, 4, 8]), op=ALU.add, R=[lg, s["gpen"]], W=[s["el"]])
            yield
            V("reduce_max", out=s["m1"][:], in_=s["el"][:], axis=AX.X, R=[s["el"]], W=[s["m1"]])
            yield
            V("tensor_scalar", out=s["k1"][:], in0=s["el"][:], scalar1=s["m1"][:, 0:1], scalar2=None, op0=ALU.is_ge,
              R=[s["el"], s["m1"]], W=[s["k1"]])
            yield
            V("scalar_tensor_tensor", out=s["el2"][:], in0=s["k1"][:], scalar=-BIG, in1=s["el"][:], op0=ALU.mult, op1=ALU.add,
              R=[s["k1"], s["el"]], W=[s["el2"]])
            yield
            V("reduce_max", out=s["m2"][:], in_=s["el2"][:], axis=AX.X, R=[s["el2"]], W=[s["m2"]])
            yield
            V("tensor_scalar", out=s["k2"][:], in0=s["el2"][:], scalar1=s["m2"][:, 0:1], scalar2=None, op0=ALU.is_ge,
              R=[s["el2"], s["m2"]], W=[s["k2"]])
            yield
            V("tensor_tensor", out=s["d"][:], in0=s["m2"][:], in1=s["m1"][:], op=ALU.subtract, R=[s["m1"], s["m2"]], W=[s["d"]])
            yield
            P.op("act", "activation", out=s["e"][:], in_=s["d"][:], func=AF.Exp, R=[s["d"]], W=[s["e"]])
            yield
            V("tensor_scalar", out=s["w1"][:], in0=s["e"][:], scalar1=1.0, scalar2=None, op0=ALU.add, R=[s["e"]], W=[s["w1"]])
            yield
            V("reciprocal", out=s["w1"][:], in_=s["w1"][:], R=[s["w1"]], W=[s["w1"]])
            yield
            V("tensor_tensor", out=s["w2"][:], in0=s["e"][:], in1=s["w1"][:], op=ALU.mult, R=[s["e"], s["w1"]], W=[s["w2"]])
            yield
            V("tensor_tensor", out=s["w1"][:], in0=s["w1"][:], in1=s["pg"][:], op=ALU.mult, R=[s["w1"], s["pg"]], W=[s["w1"]])
            yield
            V("tensor_tensor", out=s["w2"][:], in0=s["w2"][:], in1=s["pg"][:], op=ALU.mult, R=[s["w2"], s["pg"]], W=[s["w2"]])
            yield
            V("tensor_scalar", out=s["cmb"][:], in0=s["k1"][:], scalar1=s["w1"][:, 0:1], scalar2=None, op0=ALU.mult,
              R=[s["k1"], s["w1"]], W=[s["cmb"]])
            yield
            V("scalar_tensor_tensor", out=s["cmb2"][:], in0=s["k2"][:], scalar=s["w2"][:, 0:1], in1=s["cmb"][:],
              op0=ALU.mult, op1=ALU.add, R=[s["k2"], s["w2"], s["cmb"]], W=[s["cmb2"]])
            yield
            P.op("pe", "transpose", out=pc[:], in_=s["cmb2"][:], identity=idf[:], R=[s["cmb2"], idf], W=[pc])
            yield
            c_ = cT[i % 2]
            P.op("act", "copy", out=c_[:], in_=pc[:], R=[pc], W=[c_])
            yield
            P.dma("sp", D["combT"][:, t * 128:(t + 1) * 128], c_[:], R=[c_], W=[P.key("st")])
            yield

        _interleave([part_a(0)])
        for i in range(len(tiles)):
            _interleave([part_b(i), part_a(i + 1) if i + 1 < len(tiles) else None])


def stage_moe_b(P, D, l, C, groups, res="res", lat_only=False):
    with P.stage("moe_b"):
        GT = max(len(g) for g in groups)
        NTOK = GT * 128
        xT = P.sb([128, KC, NTOK], BF16)
        yacc = [P.sb([128, DM]) for _ in range(GT)]
        cb = [P.sb([128, NTOK]) for _ in range(2)]
        actT = [P.sb([128, 2, 2, NTOK], BF16) for _ in range(2)]
        w13 = [P.sb([128, 2, KC, FF], BF16) for _ in range(2)]
        w2 = [P.sb([128, 2, DM], BF16) for _ in range(4)]
        sa = [P.sb([128, 512]) for _ in range(2)]
        sab = sa
        pa = [P.ps([128, 512]) for _ in range(2)]
        pb = [P.ps([128, 512]) for _ in range(2)]
        NPO = 3
        po = [P.ps([128, 512]) for _ in range(NPO)]
        gt = P.sb([128, DM])
        xr = P.sb([128, DM])
        rk = P.key("res")
        cnt = {"p1": 0, "p2": 0}

        def p1_block(e, slot, j, h0, hw, fc):
            wa, c_ = w13[e % 2], cb[e % 2]
            k = cnt["p1"]
            cnt["p1"] += 1
            pa_, pb_, sa_, sab_ = pa[k % 2], pb[k % 2], sa[k % 2], sab[k % 2]
            for kc in range(KC):
                P.op("pe", "matmul", out=pa_[:, 0:hw], lhsT=wa[:, 0, kc, fc * 128:(fc + 1) * 128], rhs=xT[:, kc, h0:h0 + hw],
                     start=(kc == 0), stop=(kc == KC - 1), R=[wa, xT], W=[pa_], tw=(kc == KC - 1))
            for kc in range(KC):
                P.op("pe", "matmul", out=pb_[:, 0:hw], lhsT=wa[:, 1, kc, fc * 128:(fc + 1) * 128], rhs=xT[:, kc, h0:h0 + hw],
                     start=(kc == 0), stop=(kc == KC - 1), R=[wa, xT], W=[pb_], tw=(kc == KC - 1))
            P.op("act", "activation", out=sa_[:, 0:hw], in_=pa_[:, 0:hw], func=AF.Silu, R=[pa_], W=[sa_])
            P.op("dve", "tensor_tensor", out=sab_[:, 0:hw], in0=pb_[:, 0:hw], in1=sa_[:, 0:hw], op=ALU.mult, R=[pb_, sa_], W=[sab_])
            P.op("pool", "tensor_tensor", out=actT[slot][:, j, fc, h0:h0 + hw], in0=sab_[:, 0:hw], in1=c_[:, h0:h0 + hw],
                 op=ALU.mult, R=[sab_, c_], W=[actT[slot]])

        def p2_item(pair, slot, ti, cg, first):
            k = cnt["p2"]
            cnt["p2"] += 1
            pp = po[k % len(po)]
            n = 0
            for j, e in enumerate(pair):
                for fc in range(2):
                    P.op("pe", "matmul", out=pp[:], lhsT=actT[slot][:, j, fc, ti * 128:(ti + 1) * 128],
                         rhs=w2[e % 4][:, fc, cg * 512:(cg + 1) * 512], start=(n == 0), stop=(n == 3),
                         R=[actT[slot], w2[e % 4]], W=[pp], tw=(n == 3))
                    n += 1
            dst = yacc[ti][:, cg * 512:(cg + 1) * 512]
            if first:
                P.op("dve", "tensor_copy", out=dst, in_=pp[:], R=[pp], W=[yacc[ti]])
            else:
                P.op("dve", "tensor_tensor", out=dst, in0=pp[:], in1=dst, op=ALU.add, R=[pp, yacc[ti]], W=[yacc[ti]])

        for grp in groups:
            nt = len(grp)
            t0 = grp[0]
            assert grp == list(range(t0, t0 + nt))
            ntok = nt * 128
            r = 0 if lat_only else tile_r(t0)
            P.dma("sp", xT[:, :, 0:ntok], D["xn2T"][:, :, t0 * 128:t0 * 128 + ntok], W=[xT])
            halves = [(h0, min(512, ntok - h0)) for h0 in range(0, ntok, 512)]
            prev = None
            for k in range(N_EXP // 2):
                pair = (2 * k, 2 * k + 1)
                slot = k % 2
                blocks = []
                for j, e in enumerate(pair):
                    wa, wb2, c_ = w13[e % 2], w2[e % 4], cb[e % 2]
                    P.dma("pool", wa[:, 0], D["moe_w1"][l, e // 8, e % 8].rearrange("(kc p) f -> p kc f", p=128), W=[wa])
                    P.dma("pool", wa[:, 1], D["moe_w3"][l, e // 8, e % 8].rearrange("(kc p) f -> p kc f", p=128), W=[wa])
                    P.dma("pool", wb2[:], D["moe_w2"][l, e // 8, e % 8].rearrange("(fc p) d -> p fc d", p=128), W=[wb2])
                    P.dma("sp", c_[:, 0:ntok], D["combT"][e, t0 * 128:t0 * 128 + ntok].partition_broadcast(128), W=[c_])
                    for (h0, hw) in halves:
                        for fc in range(2):
                            blocks.append((e, slot, j, h0, hw, fc))
                items = []
                if prev is not None:
                    items = [(prev[0], prev[1], ti, cg, prev[2]) for ti in range(nt) for cg in range(4)]
                per = (len(items) + len(blocks) - 1) // len(blocks) if items else 0
                for bi, blk in enumerate(blocks):
                    p1_block(*blk)
                    for it in items[bi * per:(bi + 1) * per]:
                        p2_item(*it)
                prev = (pair, slot, k == 0)
            for ti in range(nt):
                for cg in range(4):
                    p2_item(prev[0], prev[1], ti, cg, prev[2])
            cur_r = None
            for ti, t in enumerate(grp):
                r_t = 0 if lat_only else tile_r(t)
                if r_t != cur_r:
                    P.dma("sp", gt[:], D["modd"][l, r_t, 5 * DM:6 * DM].partition_broadcast(128), W=[gt])
                    cur_r = r_t
                P.dma("sp", xr[:], D[res][t * 128:(t + 1) * 128, :], W=[xr])
                P.op("dve", "tensor_tensor", out=yacc[ti][:], in0=yacc[ti][:], in1=gt[:], op=ALU.mult, R=[yacc[ti], gt], W=[yacc[ti]])
                P.op("dve", "tensor_tensor", out=xr[:], in0=xr[:], in1=yacc[ti][:], op=ALU.add, R=[xr, yacc[ti]], W=[xr])
                P.dma("sp", D[res][t * 128:(t + 1) * 128, :], xr[:], R=[xr], W=[P.key("st")])


def stage_final(P, D):
    with P.stage("final_norm"):
        gb = P.sb([128, DM])
        P.dma("sp", gb[:], D["final_g"].partition_broadcast(128), W=[gb])
        xt = [P.sb([128, DM]) for _ in range(2)]
        ot = [P.sb([128, DM]) for _ in range(2)]
        junk = P.sb([128, DM])
        ss = [P.sb([128, 1]) for _ in range(2)]
        rs = [P.sb([128, 1]) for _ in range(2)]
        n = T_HALF // 128
        P.dma("sp", xt[0][:], D["res_h"][0:128, :], W=[xt[0]])
        for i in range(n):
            x_, o_, ss_, rs_ = xt[i % 2], ot[i % 2], ss[i % 2], rs[i % 2]
            if i + 1 < n:
                P.dma("sp", xt[(i + 1) % 2][:], D["res_h"][(i + 1) * 128:(i + 2) * 128, :], W=[xt[(i + 1) % 2]])
            rms_rstd(P, x_, junk, ss_, rs_, DM)
            P.op("dve", "scalar_tensor_tensor", out=o_[:], in0=x_[:], scalar=rs_[:, 0:1], in1=gb[:], op0=ALU.mult, op1=ALU.mult,
                 R=[x_, rs_, gb], W=[o_])
            P.dma("sp", D["out"][i * 128:(i + 1) * 128, :], o_[:], R=[o_], W=[P.key("st")])
LA_CFG = {"gla": dict(dk=64, ycol=512, g="gla_g", ng="gla_norm_g"),
          "ml": dict(dk=128, ycol=1024, g="ml_o", ng="ml_norm_g"),
          "ret": dict(dk=128, ycol=1536, g="ret_g", ng="ret_norm_g")}


def _interleave(gens):
    active = [g for g in gens if g is not None]
    while active:
        for g in list(active):
            try:
                next(g)
            except StopIteration:
                active.remove(g)


def stage_linattn(P, D, l, C, kind, dr, with_ctx):
    cfg = LA_CFG[kind]
    H, dk, dv, dvp = 4, cfg["dk"], 128, 128
    HK = H * dk
    qo, ko, vo, go = OFF[kind + "_q"], OFF[kind + "_k"], OFF[kind + "_v"], OFF[cfg["g"]]
    qscale = dk ** -0.5 if kind in ("gla", "ret") else 1.0
    kscale = dk ** -0.5 if kind == "ml" else 1.0
    idb, idf = C["idb"], C["idf"]
    order = ([0, 1] + list(range(2, NT))) if dr == 0 else ([1, 0] + list(range(NT - 1, 1, -1)))
    with P.stage(f"la_{kind}{dr}"):
        two = lambda shape, dt=F32: [P.sb(shape, dt) for _ in range(2)]
        tri = P.sb([128, 128])
        ones = P.sb([128, 128])
        P.dma("sp", tri[:], D["k_tri"][dr], W=[tri])
        P.dma("sp", ones[:], D["k_tri"][2], W=[ones])
        S = P.sb([dk, H, dvp])
        Sb = P.sb([dk, H, dvp], BF16)
        P.op("dve", "memset", ap=S[:], constant=0.0, W=[S])
        P.op("dve", "memset", ap=Sb[:], constant=0.0, W=[Sb])
        Vb = two([128, H, dvp], BF16)
        if kind == "ml":
            onesb = P.sb([128, 1], BF16)
            P.op("dve", "memset", ap=onesb[:], constant=1.0, W=[onesb])
            nS = P.sb([dk, H])
            nSb = P.sb([dk, H], BF16)
            P.op("dve", "memset", ap=nS[:], constant=0.0, W=[nS])
            P.op("dve", "memset", ap=nSb[:], constant=0.0, W=[nSb])
            pD = P.ps([128, 32])
        LA = two([128, HK])
        if kind == "gla":
            WA = P.sb([32, 256])
            P.op("dve", "memset", ap=WA[:], constant=0.0, W=[WA])
            P.dma("sp", WA[dr * 16:(dr + 1) * 16, :], D["gla_wa2"][l, dr], W=[WA])
            bab = P.sb([128, 256])
            P.dma("sp", bab[:], D["gla_ba"][l, dr].partition_broadcast(128), W=[bab])
            Rt = two([128, 32])
            RT = two([32, 128])
        if kind == "ml":
            bi = P.sb([128, 4])
            bf = P.sb([128, 4])
            P.dma("sp", bi[:], D["ml_gate_b"][l, dr, 0].partition_broadcast(128), W=[bi])
            P.dma("sp", bf[:], D["ml_gate_b"][l, dr, 1].partition_broadcast(128), W=[bf])
            Gt = two([128, 16])
            lf = two([128, 4])
            eig = two([128, 4])
        if kind == "ret":
            rd = P.sb([128, 4])
            P.dma("sp", rd[:], D["ret_decay"][l, dr].partition_broadcast(128), W=[rd])
            P.op("act", "activation", out=rd[:], in_=rd[:], func=AF.Exp, scale=-1.0, R=[rd], W=[rd])
            P.op("act", "activation", out=rd[:], in_=rd[:], func=AF.Ln, bias=1.0, R=[rd], W=[rd])
            P.op("dve", "tensor_scalar", out=LA[0][:].rearrange("p (h d) -> p h d", h=H),
                 in0=rd[:].unsqueeze(2).to_broadcast([128, H, dk]), scalar1=-1.0, scalar2=None, op0=ALU.mult, R=[rd], W=[LA[0]])
            cs = two([128, 64])
            sn = two([128, 64])
            rtq = [P.sb([128, H, 64]) for _ in range(4)]
            rtk = [P.sb([128, H, 64]) for _ in range(4)]
        Qt, Kt, Vt = two([128, HK]), two([128, HK]), two([128, 512])
        BCs, EQ, EK, ES = two([128, HK]), two([128, HK]), two([128, HK]), two([128, HK])
        Qin, Kin, Kst = two([128, HK], BF16), two([128, HK], BF16), two([128, HK], BF16)
        tmpk = two([128, HK])
        A = two([dk, H])
        QKT = two([dk, 2 * H, 128], BF16)
        atts = P.sb([128, H, 128], BF16)
        pBC, pBT = P.ps([128, HK]), P.ps([128, HK])
        pA = P.ps([128, 160])
        pT = P.ps([dk, 2 * H, 128], BF16)
        pAtt = P.ps([128, H, 128])
        pO = P.ps([128, H, dvp])
        pU = P.ps([dk, H, dvp])
        Osb = two([128, H, 128])
        if kind == "ml":
            dn = P.sb([128, H])
        if dr == 1:
            Oprev = two([128, H, 128])
            Gg = two([128, 512])
            gnb = P.sb([128, 512])
            P.dma("sp", gnb[:], D[cfg["ng"]][l].partition_broadcast(128), W=[gnb])
            sq = P.sb([128, H, 128])
            ssq = P.sb([128, H])
        n_it = len(order)
        hoist = {"done": False}

        def flags(it):
            t = order[it]
            is_ctx = t < 2
            return t, is_ctx, (with_ctx or not is_ctx)

        def loads(it):
            t, is_ctx, need_out = flags(it)
            b = it % 2
            rows = slice(t * 128, (t + 1) * 128)
            P.dma("sp", Qt[b][:], D["p"][rows, qo:qo + HK], W=[Qt[b]])
            P.dma("sp", Kt[b][:], D["p"][rows, ko:ko + HK], W=[Kt[b]])
            P.dma("sp", Vt[b][:], D["p"][rows, vo:vo + 512], W=[Vt[b]])
            if kind == "gla":
                P.dma("sp", Rt[b][:], D["p"][rows, OFF["gla_r"]:OFF["gla_r"] + 32], W=[Rt[b]])
            if kind == "ml":
                P.dma("sp", Gt[b][:], D["p"][rows, OFF["ml_gates"]:OFF["ml_gates"] + 16], W=[Gt[b]])
            if kind == "ret" and not is_ctx:
                lr = slice((t - 2) * 128, (t - 1) * 128)
                P.dma("sp", cs[b][:], D["k_cos"][lr, :], W=[cs[b]])
                P.dma("sp", sn[b][:], D["k_sin"][lr, :], W=[sn[b]])

        def loads_post(it):
            t, is_ctx, need_out = flags(it)
            b = it % 2
            rows = slice(t * 128, (t + 1) * 128)
            if dr == 1 and need_out:
                P.dma("sp", Oprev[b][:], D["oacc"][rows, :].rearrange("p (h d) -> p h d", h=H), W=[Oprev[b]])
                P.dma("sp", Gg[b][:], D["p"][rows, go:go + 512], W=[Gg[b]])

        def prep(it):
            t, is_ctx, need_out = flags(it)
            b = it % 2
            c = 0 if kind == "ret" else b
            Q, K, V = Qt[b], Kt[b], Vt[b]
            LA_, BCs_, EQ_, EK_, ES_, A_ = LA[c], BCs[c], EQ[c], EK[c], ES[c], A[c]
            if kind == "gla":
                R_, RT_ = Rt[b], RT[b]
                P.op("pe", "transpose", out=pA[0:32, 32:160], in_=R_[:], identity=idf[:], R=[R_, idf], W=[pA])
                yield
                P.op("act", "copy", out=RT_[:], in_=pA[0:32, 32:160], R=[pA], W=[RT_])
                yield
                P.op("pe", "matmul", out=pBC[:], lhsT=RT_[:], rhs=WA[:], start=True, stop=True, R=[RT_, WA], W=[pBC])
                yield
                P.op("dve", "tensor_tensor", out=LA_[:], in0=pBC[:], in1=bab[:], op=ALU.add, R=[pBC, bab], W=[LA_])
                yield
                P.op("act", "activation", out=LA_[:], in_=LA_[:], func=AF.Exp, scale=-1.0, R=[LA_], W=[LA_])
                yield
                P.op("act", "activation", out=LA_[:], in_=LA_[:], func=AF.Ln, bias=1.0, R=[LA_], W=[LA_])
                yield
                P.op("dve", "tensor_scalar", out=LA_[:], in0=LA_[:], scalar1=-1.0 / 16.0, scalar2=None, op0=ALU.mult, R=[LA_], W=[LA_])
                yield
            if kind == "ml":
                G4, eig_, lf_ = Gt[b], eig[b], lf[b]
                P.op("dve", "tensor_tensor", out=eig_[:], in0=G4[:, dr * 8:dr * 8 + 4], in1=bi[:], op=ALU.add, R=[G4, bi], W=[eig_])
                yield
                P.op("act", "activation", out=eig_[:], in_=eig_[:], func=AF.Exp, R=[eig_], W=[eig_])
                yield
                P.op("dve", "tensor_tensor", out=lf_[:], in0=G4[:, dr * 8 + 4:dr * 8 + 8], in1=bf[:], op=ALU.add, R=[G4, bf], W=[lf_])
                yield
                P.op("act", "activation", out=lf_[:], in_=lf_[:], func=AF.Exp, scale=-1.0, R=[lf_], W=[lf_])
                yield
                P.op("act", "activation", out=lf_[:], in_=lf_[:], func=AF.Ln, bias=1.0, R=[lf_], W=[lf_])
                yield
                P.op("dve", "tensor_scalar", out=LA_[:].rearrange("p (h d) -> p h d", h=H),
                     in0=lf_[:].unsqueeze(2).to_broadcast([128, H, dk]), scalar1=-1.0, scalar2=None, op0=ALU.mult, R=[lf_], W=[LA_])
                yield
            if not (kind == "ret" and hoist["done"]):
                P.op("pe", "matmul", out=pBC[:], lhsT=tri[:], rhs=LA_[:], start=True, stop=True, R=[tri, LA_], W=[pBC])
                P.op("pe", "matmul", out=pBT[:], lhsT=ones[:], rhs=LA_[:], start=True, stop=True, R=[ones, LA_], W=[pBT])
                for h in range(H):
                    P.op("pe", "matmul", out=pA[0:dk, h:h + 1], lhsT=LA_[:, h * dk:(h + 1) * dk], rhs=ones[:, 0:1], start=True, stop=True,
                         R=[LA_, ones], W=[pA])
                yield
                P.op("act", "copy", out=BCs_[:], in_=pBC[:], R=[pBC], W=[BCs_])
                yield
                P.op("act", "activation", out=EQ_[:], in_=BCs_[:], func=AF.Exp, R=[BCs_], W=[EQ_])
                yield
                P.op("dve", "tensor_tensor", out=ES_[:], in0=pBT[:], in1=BCs_[:], op=ALU.subtract, R=[pBT, BCs_], W=[ES_])
                yield
                P.op("act", "activation", out=EK_[:], in_=BCs_[:], func=AF.Exp, scale=-1.0, R=[BCs_], W=[EK_])
                yield
                P.op("act", "activation", out=ES_[:], in_=ES_[:], func=AF.Exp, R=[ES_], W=[ES_])
                yield
                P.op("act", "activation", out=A_[:], in_=pA[0:dk, 0:H], func=AF.Exp, R=[pA], W=[A_])
                yield
                hoist["done"] = True
            if kind == "ret" and not is_ctx:
                c_, s_ = cs[b], sn[b]
                cb_ = c_[:].unsqueeze(1).to_broadcast([128, H, 64])
                sb_ = s_[:].unsqueeze(1).to_broadcast([128, H, 64])
                for X, eng, rt in ((Q, "dve", rtq), (K, "pool", rtk)):
                    X3 = X[:].rearrange("p (h d) -> p h d", h=H)
                    a1, a2 = X3[:, :, 0:64], X3[:, :, 64:128]
                    t1, t2, t3, t4 = rt
                    P.op(eng, "tensor_tensor", out=t1[:], in0=a1, in1=cb_, op=ALU.mult, R=[X, c_], W=[t1])
                    P.op(eng, "tensor_tensor", out=t2[:], in0=a2, in1=sb_, op=ALU.mult, R=[X, s_], W=[t2])
                    P.op(eng, "tensor_tensor", out=t3[:], in0=a1, in1=sb_, op=ALU.mult, R=[X, s_], W=[t3])
                    P.op(eng, "tensor_tensor", out=t4[:], in0=a2, in1=cb_, op=ALU.mult, R=[X, c_], W=[t4])
                    yield
                    P.op(eng, "tensor_tensor", out=a1, in0=t1[:], in1=t2[:], op=ALU.subtract, R=[t1, t2], W=[X])
                    P.op(eng, "tensor_tensor", out=a2, in0=t3[:], in1=t4[:], op=ALU.add, R=[t3, t4], W=[X])
                    yield
            P.op("dve", "scalar_tensor_tensor", out=Qin[b][:], in0=Q[:], scalar=qscale, in1=EQ_[:], op0=ALU.mult, op1=ALU.mult,
                 R=[Q, EQ_], W=[Qin[b]])
            yield
            if kind == "ml":
                e3 = eig[b][:].unsqueeze(2).to_broadcast([128, H, dk])
                P.op("dve", "scalar_tensor_tensor", out=tmpk[0][:], in0=K[:], scalar=kscale, in1=EK_[:], op0=ALU.mult, op1=ALU.mult,
                     R=[K, EK_], W=[tmpk[0]])
                P.op("dve", "tensor_tensor", out=Kin[b][:].rearrange("p (h d) -> p h d", h=H),
                     in0=tmpk[0][:].rearrange("p (h d) -> p h d", h=H), in1=e3, op=ALU.mult, R=[tmpk[0], eig[b]], W=[Kin[b]])
                yield
                P.op("dve", "scalar_tensor_tensor", out=tmpk[1][:], in0=K[:], scalar=kscale, in1=ES_[:], op0=ALU.mult, op1=ALU.mult,
                     R=[K, ES_], W=[tmpk[1]])
                P.op("dve", "tensor_tensor", out=Kst[b][:].rearrange("p (h d) -> p h d", h=H),
                     in0=tmpk[1][:].rearrange("p (h d) -> p h d", h=H), in1=e3, op=ALU.mult, R=[tmpk[1], eig[b]], W=[Kst[b]])
                yield
            else:
                P.op("dve", "tensor_tensor", out=Kin[b][:], in0=K[:], in1=EK_[:], op=ALU.mult, R=[K, EK_], W=[Kin[b]])
                yield
                P.op("dve", "tensor_tensor", out=Kst[b][:], in0=K[:], in1=ES_[:], op=ALU.mult, R=[K, ES_], W=[Kst[b]])
                yield
            P.op("act", "copy", out=Vb[b][:], in_=V[:].rearrange("p (h d) -> p h d", h=H), R=[V], W=[Vb[b]])
            yield
            if need_out:
                for h in range(H):
                    P.op("pe", "transpose", out=pT[:, h, :], in_=Qin[b][:, h * dk:(h + 1) * dk], identity=idb[:], R=[Qin[b], idb], W=[pT])
                    P.op("pe", "transpose", out=pT[:, H + h, :], in_=Kin[b][:, h * dk:(h + 1) * dk], identity=idb[:], R=[Kin[b], idb], W=[pT])
                yield
                P.op("act", "copy", out=QKT[b][:], in_=pT[:], R=[pT], W=[QKT[b]])
                yield

        def heads(it):
            t, is_ctx, need_out = flags(it)
            b = it % 2
            c = 0 if kind == "ret" else b
            rows = slice(t * 128, (t + 1) * 128)
            QKT_, Vb_, Kst_, A_ = QKT[b], Vb[b], Kst[b], A[c]
            O_ = Osb[b]
            if need_out:
                for h in range(H):
                    P.op("pe", "matmul", out=pAtt[:, h, :], lhsT=QKT_[:, H + h, :], rhs=QKT_[:, h, :], start=True, stop=True, R=[QKT_], W=[pAtt])
                yield
                P.op("dve", "tensor_tensor", out=atts[:], in0=pAtt[:], in1=tri[:].unsqueeze(1).to_broadcast([128, H, 128]), op=ALU.mult,
                     R=[pAtt, tri], W=[atts])
                yield
                for h in range(H):
                    P.op("pe", "matmul", out=pO[:, h, :], lhsT=atts[:, h, :], rhs=Vb_[:, h, :], start=True, stop=False,
                         R=[atts, Vb_], W=[pO], tw=False)
                    P.op("pe", "matmul", out=pO[:, h, :], lhsT=QKT_[:, h, :], rhs=Sb[:, h, :], start=False, stop=True,
                         R=[QKT_, Sb], W=[pO])
                yield
                if kind == "ml":
                    for h in range(H):
                        P.op("pe", "matmul", out=pD[:, h:h + 1], lhsT=atts[:, h, :], rhs=onesb[:, 0:1], start=True, stop=False,
                             R=[atts, onesb], W=[pD], tw=False)
                        P.op("pe", "matmul", out=pD[:, h:h + 1], lhsT=QKT_[:, h, :], rhs=nSb[:, h:h + 1], start=False, stop=True,
                             R=[QKT_, nSb], W=[pD])
                    yield
            for h in range(H):
                P.op("pe", "matmul", out=pU[:, h, :], lhsT=Kst_[:, h * dk:(h + 1) * dk], rhs=Vb_[:, h, :], start=True, stop=True,
                     R=[Kst_, Vb_], W=[pU])
            yield
            if kind == "ml":
                for h in range(H):
                    P.op("pe", "matmul", out=pD[0:dk, 8 + h:9 + h], lhsT=Kst_[:, h * dk:(h + 1) * dk], rhs=onesb[:, 0:1], start=True, stop=True,
                         R=[Kst_, onesb], W=[pD])
                yield
            if need_out:
                if kind == "ml":
                    P.op("act", "activation", out=dn[:], in_=pD[:, 0:4], func=AF.Abs, R=[pD], W=[dn])
                    yield
                    P.op("dve", "tensor_scalar_max", out=dn[:], in0=dn[:], scalar1=1.0, R=[dn], W=[dn])
                    P.op("dve", "reciprocal", out=dn[:], in_=dn[:], R=[dn], W=[dn])
                    P.op("dve", "tensor_tensor", out=O_[:], in0=pO[:], in1=dn[:].unsqueeze(2).to_broadcast([128, H, 128]), op=ALU.mult,
                         R=[pO, dn], W=[O_])
                    yield
                else:
                    P.op("act", "copy", out=O_[:], in_=pO[:], R=[pO], W=[O_])
                    yield
            P.op("dve", "tensor_tensor", out=S[:], in0=S[:], in1=A_[:].unsqueeze(2).to_broadcast([dk, H, dvp]), op=ALU.mult, R=[S, A_], W=[S])
            P.op("dve", "tensor_tensor", out=S[:], in0=pU[:], in1=S[:], op=ALU.add, R=[pU, S], W=[S])
            yield
            P.op("act", "copy", out=Sb[:], in_=S[:], R=[S], W=[Sb])
            yield
            if kind == "ml":
                P.op("dve", "tensor_tensor", out=nS[:], in0=nS[:], in1=A_[:], op=ALU.mult, R=[nS, A_], W=[nS])
                P.op("dve", "tensor_tensor", out=nS[:], in0=pD[0:dk, 8:12], in1=nS[:], op=ALU.add, R=[pD, nS], W=[nS])
                yield
                P.op("act", "copy", out=nSb[:], in_=nS[:], R=[nS], W=[nSb])
                yield
            if not need_out:
                return
            if dr == 0:
                P.dma("sp", D["oacc"][rows, :].rearrange("p (h d) -> p h d", h=H), O_[:], R=[O_], W=[P.key("st")])
                return
            Op_, G_ = Oprev[b], Gg[b]
            P.op("dve", "tensor_tensor", out=O_[:], in0=O_[:], in1=Op_[:], op=ALU.add, R=[O_, Op_], W=[O_])
            yield
            if kind == "ml":
                P.op("act", "activation", out=G_[:], in_=G_[:], func=AF.Sigmoid, R=[G_], W=[G_])
                yield
                P.op("dve", "tensor_tensor", out=O_[:], in0=O_[:], in1=G_[:].rearrange("p (h d) -> p h d", h=H), op=ALU.mult,
                     R=[O_, G_], W=[O_])
                yield
            else:
                P.op("act", "activation", out=G_[:], in_=G_[:], func=AF.Silu, R=[G_], W=[G_])
                yield
            P.op("dve", "tensor_tensor", out=sq[:], in0=O_[:], in1=O_[:], op=ALU.mult, R=[O_], W=[sq])
            P.op("dve", "reduce_sum", out=ssq[:], in_=sq[:], axis=AX.X, R=[sq], W=[ssq])
            P.op("dve", "tensor_scalar", out=ssq[:], in0=ssq[:], scalar1=1.0 / 128, scalar2=EPS, op0=ALU.mult, op1=ALU.add,
                 R=[ssq], W=[ssq])
            yield
            P.op("act", "activation", out=ssq[:], in_=ssq[:], func=AF.Sqrt, R=[ssq], W=[ssq])
            yield
            P.op("dve", "reciprocal", out=ssq[:], in_=ssq[:], R=[ssq], W=[ssq])
            P.op("dve", "tensor_tensor", out=O_[:], in0=O_[:], in1=ssq[:].unsqueeze(2).to_broadcast([128, H, 128]), op=ALU.mult,
                 R=[O_, ssq], W=[O_])
            yield
            P.op("dve", "tensor_tensor", out=O_[:], in0=O_[:], in1=gnb[:].rearrange("p (h d) -> p h d", h=H), op=ALU.mult,
                 R=[O_, gnb], W=[O_])
            if kind != "ml":
                P.op("dve", "tensor_tensor", out=O_[:], in0=O_[:], in1=G_[:].rearrange("p (h d) -> p h d", h=H), op=ALU.mult,
                     R=[O_, G_], W=[O_])
            yield
            P.dma("sp", D["ymix"][rows, cfg["ycol"]:cfg["ycol"] + 512].rearrange("p (h d) -> p h d", h=H), O_[:], R=[O_],
                  W=[P.key("st")])

        loads(0)
        if n_it > 1:
            loads(1)
        loads_post(0)
        _interleave([prep(0)])
        for it in range(n_it):
            if it + 2 < n_it:
                loads(it + 2)
            if it + 1 < n_it:
                loads_post(it + 1)
            _interleave([heads(it), prep(it + 1) if it + 1 < n_it else None])
import math
TWO_PI = 2.0 * math.pi


def hy_dims(L):
    nkt = (L + 1 + 127) // 128
    return L // 128, nkt


def hy_tables(L):
    import ml_dtypes
    N = 2 * L
    ntl, nkt = hy_dims(L)
    n = np.arange(L, dtype=np.int64)[:, None]
    k = np.arange(nkt * 128, dtype=np.int64)[None, :]
    ang = (n * k % N).astype(np.float64) * (TWO_PI / N)
    valid = (k <= L)
    Cf = np.where(valid, np.cos(ang), 0.0)
    Sf = np.where(valid, np.sin(ang), 0.0)
    w = np.where((k == 0) | (k == L), 1.0 / N, 2.0 / N) * valid
    Ci = (Cf * w).T
    Si = (Sf * w).T
    bf = ml_dtypes.bfloat16
    pos = np.arange(L, dtype=np.float32)
    t = pos / np.float32(L - 1)
    fr = np.linspace(1e-4, 15, 16, dtype=np.float32)
    a32 = (np.float32(2.0 * math.pi / L) * pos[:, None] * fr[None, :]).astype(np.float32)
    z = np.concatenate([t[:, None], np.cos(a32), -np.sin(a32)], -1).astype(np.float32)
    tcol = np.ascontiguousarray((-t).reshape(ntl, 128).T).astype(np.float32)
    m0 = np.ones((128, 1), np.float32)
    m0[0, 0] = 0.0
    return {f"k_hyCf{L}": Cf.astype(bf), f"k_hySf{L}": Sf.astype(bf), f"k_hyCi{L}": np.ascontiguousarray(Ci).astype(bf),
            f"k_hySi{L}": np.ascontiguousarray(Si).astype(bf), f"k_hyz{L}": np.ascontiguousarray(z.T),
            f"k_hynt{L}": tcol, "k_m0": m0}


def hy_spec(L):
    ntl, nkt = hy_dims(L)
    return {f"k_hyCf{L}": ((L, nkt * 128), BF16, "ExternalInput"), f"k_hySf{L}": ((L, nkt * 128), BF16, "ExternalInput"),
            f"k_hyCi{L}": ((nkt * 128, L), BF16, "ExternalInput"), f"k_hySi{L}": ((nkt * 128, L), BF16, "ExternalInput"),
            f"k_hyz{L}": ((33, L), F32, "ExternalInput"), f"k_hynt{L}": ((128, ntl), F32, "ExternalInput"),
            "k_m0": ((128, 1), F32, "ExternalInput"),
            f"hyhsd{L}": ((2, L, 1024), BF16, "Internal"), f"hyH{L}": ((2, 2, nkt * 128, 512), F32, "Internal")}


HY_SPEC = {"hyu": ((T_ALL, 1536), F32, "Internal"), "hyz1": ((T_ALL, 512), F32, "Internal")}
HY_SPEC.update(hy_spec(T_LAT))
HY_SPEC.update(hy_spec(T_CTX))


def stage_hy_conv(P, D, l, tiles):
    with P.stage("hy_conv"):
        W = 1536
        wt = [P.sb([128, W]) for _ in range(4)]
        for k in range(3):
            P.dma("sp", wt[k][:], D["hy_conv_w"][l, k].partition_broadcast(128), W=[wt[k]])
        P.dma("sp", wt[3][:], D["hy_conv_b"][l].partition_broadcast(128), W=[wt[3]])
        um = [P.sb([128, W]) for _ in range(2)]
        uc = [P.sb([128, W]) for _ in range(2)]
        up = [P.sb([128, W]) for _ in range(2)]
        acc = [P.sb([128, W]) for _ in range(2)]
        tmp = P.sb([128, W])
        uk = P.key("hyu")
        def loads(i):
            t = tiles[i]
            r0 = t * 128
            a, b, c = um[i % 2], uc[i % 2], up[i % 2]
            first = t in (0, 2)
            last = t in (1, NT - 1)
            P.dma("sp", b[:], D["p"][r0:r0 + 128, 0:W], W=[b])
            if first:
                P.op("dve", "memset", ap=a[:], constant=0.0, W=[a])
                P.dma("sp", a[1:128, :], D["p"][r0:r0 + 127, 0:W], W=[a])
            else:
                P.dma("sp", a[:], D["p"][r0 - 1:r0 + 127, 0:W], W=[a])
            if last:
                P.op("dve", "memset", ap=c[:], constant=0.0, W=[c])
                P.dma("sp", c[0:127, :], D["p"][r0 + 1:r0 + 128, 0:W], W=[c])
            else:
                P.dma("sp", c[:], D["p"][r0 + 1:r0 + 129, 0:W], W=[c])

        loads(0)
        for i, t in enumerate(tiles):
            r0 = t * 128
            a, b, c, o = um[i % 2], uc[i % 2], up[i % 2], acc[i % 2]
            if i + 1 < len(tiles):
                loads(i + 1)
            P.op("dve", "tensor_tensor", out=o[:], in0=a[:], in1=wt[0][:], op=ALU.mult, R=[a, wt[0]], W=[o])
            P.op("dve", "tensor_tensor", out=tmp[:], in0=b[:], in1=wt[1][:], op=ALU.mult, R=[b, wt[1]], W=[tmp])
            P.op("dve", "tensor_tensor", out=o[:], in0=o[:], in1=tmp[:], op=ALU.add, R=[o, tmp], W=[o])
            P.op("dve", "tensor_tensor", out=tmp[:], in0=c[:], in1=wt[2][:], op=ALU.mult, R=[c, wt[2]], W=[tmp])
            P.op("dve", "tensor_tensor", out=o[:], in0=o[:], in1=tmp[:], op=ALU.add, R=[o, tmp], W=[o])
            P.op("dve", "tensor_tensor", out=o[:], in0=o[:], in1=wt[3][:], op=ALU.add, R=[o, wt[3]], W=[o])
            P.dma("sp", D["hyu"][r0:r0 + 128, :], o[:], R=[o], W=[P.key("st")])


def stage_hy_filt(P, D, l, L):
    ntl, nkt = hy_dims(L)
    with P.stage(f"hy_filt{L}"):
        zT = P.sb([33, L])
        P.dma("sp", zT[:], D[f"k_hyz{L}"], W=[zT])
        w1 = P.sb([33, 64])
        w2 = P.sb([64, 64])
        w3 = P.sb([64, 2048])
        P.dma("sp", w1[:], D["hy_f_w1"][l], W=[w1])
        P.dma("sp", w2[:], D["hy_f_w2"][l], W=[w2])
        P.dma("sp", w3[:], D["hy_f_w3"][l], W=[w3])
        cols = P.sb([64, 4])
        P.dma("sp", cols[:, 0:1], D["hy_f_b1"][l].rearrange("(p o) -> p o", o=1), W=[cols])
        P.dma("sp", cols[:, 1:2], D["hy_f_b2"][l].rearrange("(p o) -> p o", o=1), W=[cols])
        P.dma("sp", cols[:, 2:3], D["hy_f_freq"][l, 0].rearrange("(p o) -> p o", o=1), W=[cols])
        P.dma("sp", cols[:, 3:4], D["hy_f_freq"][l, 1].rearrange("(p o) -> p o", o=1), W=[cols])
        ntc = P.sb([128, ntl])
        P.dma("sp", ntc[:], D[f"k_hynt{L}"], W=[ntc])
        m0 = P.sb([128, 1])
        P.dma("sp", m0[:], D["k_m0"], W=[m0])
        dec = P.sb([128, 2048])
        P.dma("sp", dec[:], D["hy_decay"][l].partition_broadcast(128), W=[dec])
        P.op("act", "activation", out=dec[:], in_=dec[:], func=AF.Abs, R=[dec], W=[dec])
        h1 = P.sb([64, L])
        h2 = P.sb([64, L])
        arg = P.sb([64, 512])
        arg2 = P.sb([64, 512])
        pm = [P.ps([64, 512]) for _ in range(2)]
        CW = min(512, L)

        def sin_layer(src_ps, dst, bi, fi, dbuf):
            P.op("dve", "tensor_scalar", out=arg[:, 0:CW], in0=src_ps[:, 0:CW], scalar1=cols[:, bi:bi + 1], scalar2=cols[:, fi:fi + 1],
                 op0=ALU.add, op1=ALU.mult, R=[src_ps, cols], W=[arg])
            P.op("dve", "tensor_scalar", out=arg2[:, 0:CW], in0=arg[:, 0:CW], scalar1=math.pi, scalar2=-TWO_PI, op0=ALU.is_gt, op1=ALU.mult,
                 R=[arg], W=[arg2])
            P.op("dve", "tensor_tensor", out=arg[:, 0:CW], in0=arg[:, 0:CW], in1=arg2[:, 0:CW], op=ALU.add, R=[arg, arg2], W=[arg])
            P.op("dve", "tensor_scalar", out=arg2[:, 0:CW], in0=arg[:, 0:CW], scalar1=-math.pi, scalar2=TWO_PI, op0=ALU.is_lt, op1=ALU.mult,
                 R=[arg], W=[arg2])
            P.op("dve", "tensor_tensor", out=arg[:, 0:CW], in0=arg[:, 0:CW], in1=arg2[:, 0:CW], op=ALU.add, R=[arg, arg2], W=[arg])
            P.op("dve", "tensor_scalar", out=arg[:, 0:CW], in0=arg[:, 0:CW], scalar1=3.141592, scalar2=-3.141592, op0=ALU.min, op1=ALU.max,
                 R=[arg], W=[arg])
            P.op("act", "activation", out=dst, in_=arg[:, 0:CW], func=AF.Sin, R=[arg], W=[dbuf])

        for ci in range(L // CW):
            cs = slice(ci * CW, (ci + 1) * CW)
            pp = pm[ci % 2]
            P.op("pe", "matmul", out=pp[:, 0:CW], lhsT=w1[:], rhs=zT[:, cs], start=True, stop=True, R=[w1, zT], W=[pp])
            sin_layer(pp, h1[:, cs], 0, 2, h1)
        for ci in range(L // CW):
            cs = slice(ci * CW, (ci + 1) * CW)
            pp = pm[ci % 2]
            P.op("pe", "matmul", out=pp[:, 0:CW], lhsT=w2[:], rhs=h1[:, cs], start=True, stop=True, R=[w2, h1], W=[pp])
            sin_layer(pp, h2[:, cs], 1, 3, h2)
        ph = [P.ps([128, 512]) for _ in range(4)]
        env = [P.sb([128, 2048]) for _ in range(2)]
        hh = [P.sb([128, 2048]) for _ in range(2)]
        hs = [P.sb([128, 1024], BF16) for _ in range(2)]
        hd = [P.sb([128, 1024], BF16) for _ in range(2)]
        hk = P.key("hyhsd")
        for tt in range(ntl):
            e_, h_, s_, d_ = env[tt % 2], hh[tt % 2], hs[tt % 2], hd[tt % 2]
            P.op("act", "activation", out=e_[:], in_=dec[:], func=AF.Exp, scale=ntc[:, tt:tt + 1], R=[dec, ntc], W=[e_])
            for q in range(4):
                P.op("pe", "matmul", out=ph[q][:], lhsT=h2[:, tt * 128:(tt + 1) * 128], rhs=w3[:, q * 512:(q + 1) * 512],
                     start=True, stop=True, R=[h2, w3], W=[ph[q]])
                P.op("dve", "tensor_tensor", out=h_[:, q * 512:(q + 1) * 512], in0=ph[q][:], in1=e_[:, q * 512:(q + 1) * 512],
                     op=ALU.mult, R=[ph[q], e_], W=[h_])
            for o in range(2):
                f_ = h_[:, o * 1024:o * 1024 + 512]
                b_ = h_[:, o * 1024 + 512:(o + 1) * 1024]
                if tt == 0:
                    P.op("dve", "tensor_scalar", out=b_, in0=b_, scalar1=m0[:, 0:1], scalar2=None, op0=ALU.mult, R=[h_, m0], W=[h_])
                P.op("dve", "tensor_tensor", out=s_[:, o * 512:(o + 1) * 512], in0=f_, in1=b_, op=ALU.add, R=[h_], W=[s_])
                P.op("dve", "tensor_tensor", out=d_[:, o * 512:(o + 1) * 512], in0=b_, in1=f_, op=ALU.subtract, R=[h_], W=[d_])
            P.dma("sp", D[f"hyhsd{L}"][0, tt * 128:(tt + 1) * 128, :], s_[:], R=[s_], W=[P.key("st")])
            P.dma("sp", D[f"hyhsd{L}"][1, tt * 128:(tt + 1) * 128, :], d_[:], R=[d_], W=[P.key("st")])


def stage_hy_filt_fft(P, D, L):
    ntl, nkt = hy_dims(L)
    with P.stage(f"hy_hfft{L}"):
        HS = P.sb([128, ntl, 1024], BF16)
        HDd = P.sb([128, ntl, 1024], BF16)
        half = max(1, ntl // 2)
        for a in range(0, ntl, half):
            P.dma("sp", HS[:, a:a + half, :], D[f"hyhsd{L}"][0, a * 128:(a + half) * 128, :].rearrange("(nt p) c -> p nt c", p=128), W=[HS])
            P.dma("sp", HDd[:, a:a + half, :], D[f"hyhsd{L}"][1, a * 128:(a + half) * 128, :].rearrange("(nt p) c -> p nt c", p=128), W=[HDd])
        tc_ = [P.sb([128, ntl, 128], BF16) for _ in range(2)]
        ts_ = [P.sb([128, ntl, 128], BF16) for _ in range(2)]
        pp = [P.ps([128, 512]) for _ in range(4)]
        ob = [P.sb([128, 512]) for _ in range(4)]
        Hk = P.key("hyH")
        def tloads(kt):
            P.dma("sp", tc_[kt % 2][:], D[f"k_hyCf{L}"][:, kt * 128:(kt + 1) * 128].rearrange("(nt p) k -> p nt k", p=128), W=[tc_[kt % 2]])
            P.dma("sp", ts_[kt % 2][:], D[f"k_hySf{L}"][:, kt * 128:(kt + 1) * 128].rearrange("(nt p) k -> p nt k", p=128), W=[ts_[kt % 2]])

        tloads(0)
        for kt in range(nkt):
            c_, s_ = tc_[kt % 2], ts_[kt % 2]
            if kt + 1 < nkt:
                tloads(kt + 1)
            for o in range(2):
                for ri, (tab, src) in enumerate(((c_, HS), (s_, HDd))):
                    acc = pp[o * 2 + ri]
                    for nt in range(ntl):
                        P.op("pe", "matmul", out=acc[:], lhsT=tab[:, nt, :], rhs=src[:, nt, o * 512:(o + 1) * 512], start=(nt == 0),
                             stop=(nt == ntl - 1), R=[tab, src], W=[acc], tw=(nt == ntl - 1))
                    o_ = ob[o * 2 + ri]
                    if ri == 0:
                        P.op("act", "copy", out=o_[:], in_=acc[:], R=[acc], W=[o_])
                    else:
                        P.op("dve", "tensor_copy", out=o_[:], in_=acc[:], R=[acc], W=[o_])
                    P.dma("sp", D[f"hyH{L}"][o, ri, kt * 128:(kt + 1) * 128, :], o_[:], R=[o_], W=[P.key("st")])


def stage_hy_lconv(P, D, l, L, o, row0):
    ntl, nkt = hy_dims(L)
    zsrc = D["hyu"][row0:row0 + L, 0:512] if o == 0 else D["hyz1"][row0:row0 + L, :]
    gsrc = D["hyu"][row0:row0 + L, (o + 1) * 512:(o + 2) * 512]
    dst = D["hyz1"][row0:row0 + L, :] if o == 0 else D["ymix"][row0:row0 + L, 0:512]
    with P.stage(f"hy_lconv{L}_{o}"):
        Z = P.sb([128, ntl, 512], BF16)
        half = max(1, ntl // 2)
        for a in range(0, ntl, half):
            P.dma("pool", Z[:, a:a + half, :], zsrc[a * 128:(a + half) * 128, :].rearrange("(nt p) c -> p nt c", p=128), W=[Z])
        Y = P.sb([128, nkt, 2, 512], BF16)
        tc_ = [P.sb([128, ntl, 128], BF16) for _ in range(2)]
        ts_ = [P.sb([128, ntl, 128], BF16) for _ in range(2)]
        pX = [P.ps([128, 512]) for _ in range(4)]
        Xr, Xi = [P.sb([128, 512]) for _ in range(2)], [P.sb([128, 512]) for _ in range(2)]
        Hr, Hi = [P.sb([128, 512]) for _ in range(2)], [P.sb([128, 512]) for _ in range(2)]
        t1, t2, t3, t4 = P.sb([128, 512]), P.sb([128, 512]), P.sb([128, 512]), P.sb([128, 512])
        for kt in range(nkt):
            c_, s_ = tc_[kt % 2], ts_[kt % 2]
            P.dma("sp", c_[:], D[f"k_hyCf{L}"][:, kt * 128:(kt + 1) * 128].rearrange("(nt p) k -> p nt k", p=128), W=[c_])
            P.dma("sp", s_[:], D[f"k_hySf{L}"][:, kt * 128:(kt + 1) * 128].rearrange("(nt p) k -> p nt k", p=128), W=[s_])
            hr, hi, xr, xi = Hr[kt % 2], Hi[kt % 2], Xr[kt % 2], Xi[kt % 2]
            P.dma("sp", hr[:], D[f"hyH{L}"][o, 0, kt * 128:(kt + 1) * 128, :], W=[hr])
            P.dma("sp", hi[:], D[f"hyH{L}"][o, 1, kt * 128:(kt + 1) * 128, :], W=[hi])
            pr, pi = pX[(kt % 2) * 2], pX[(kt % 2) * 2 + 1]
            for nt in range(ntl):
                P.op("pe", "matmul", out=pr[:], lhsT=c_[:, nt, :], rhs=Z[:, nt, :], start=(nt == 0), stop=(nt == ntl - 1),
                     R=[c_, Z], W=[pr], tw=(nt == ntl - 1))
            for nt in range(ntl):
                P.op("pe", "matmul", out=pi[:], lhsT=s_[:, nt, :], rhs=Z[:, nt, :], start=(nt == 0), stop=(nt == ntl - 1),
                     R=[s_, Z], W=[pi], tw=(nt == ntl - 1))
            P.op("act", "copy", out=xr[:], in_=pr[:], R=[pr], W=[xr])
            P.op("act", "copy", out=xi[:], in_=pi[:], R=[pi], W=[xi])
            P.op("dve", "tensor_tensor", out=t1[:], in0=xr[:], in1=hr[:], op=ALU.mult, R=[xr, hr], W=[t1])
            P.op("dve", "tensor_tensor", out=t2[:], in0=xi[:], in1=hi[:], op=ALU.mult, R=[xi, hi], W=[t2])
            P.op("dve", "tensor_tensor", out=Y[:, kt, 0, :], in0=t1[:], in1=t2[:], op=ALU.add, R=[t1, t2], W=[Y])
            P.op("pool", "tensor_tensor", out=t3[:], in0=xi[:], in1=hr[:], op=ALU.mult, R=[xi, hr], W=[t3])
            P.op("pool", "tensor_tensor", out=t4[:], in0=xr[:], in1=hi[:], op=ALU.mult, R=[xr, hi], W=[t4])
            P.op("pool", "tensor_tensor", out=Y[:, kt, 1, :], in0=t3[:], in1=t4[:], op=ALU.subtract, R=[t3, t4], W=[Y])
        ic_ = [P.sb([128, nkt, 128], BF16) for _ in range(2)]
        is_ = [P.sb([128, nkt, 128], BF16) for _ in range(2)]
        skb = P.sb([128, 512])
        P.dma("sp", skb[:], D["hy_skip"][l, o].partition_broadcast(128), W=[skb])
        zt = [P.sb([128, 512]) for _ in range(2)]
        gt = [P.sb([128, 512]) for _ in range(2)]
        ot = [P.sb([128, 512]) for _ in range(2)]
        dk_ = P.key("hydst")
        def iloads(nt):
            P.dma("sp", ic_[nt % 2][:], D[f"k_hyCi{L}"][:, nt * 128:(nt + 1) * 128].rearrange("(kt p) n -> p kt n", p=128), W=[ic_[nt % 2]])
            P.dma("sp", is_[nt % 2][:], D[f"k_hySi{L}"][:, nt * 128:(nt + 1) * 128].rearrange("(kt p) n -> p kt n", p=128), W=[is_[nt % 2]])
            P.dma("sp", zt[nt % 2][:], zsrc[nt * 128:(nt + 1) * 128, :], W=[zt[nt % 2]])
            P.dma("sp", gt[nt % 2][:], gsrc[nt * 128:(nt + 1) * 128, :], W=[gt[nt % 2]])

        iloads(0)
        for nt in range(ntl):
            c_, s_, z_, g_, o_ = ic_[nt % 2], is_[nt % 2], zt[nt % 2], gt[nt % 2], ot[nt % 2]
            if nt + 1 < ntl:
                iloads(nt + 1)
            py = pX[nt % 2]
            for kt in range(nkt):
                P.op("pe", "matmul", out=py[:], lhsT=c_[:, kt, :], rhs=Y[:, kt, 0, :], start=(kt == 0), stop=False,
                     R=[c_, Y], W=[py], tw=False)
            for kt in range(nkt):
                P.op("pe", "matmul", out=py[:], lhsT=s_[:, kt, :], rhs=Y[:, kt, 1, :], start=False, stop=(kt == nkt - 1),
                     R=[s_, Y], W=[py], tw=(kt == nkt - 1))
            P.op("pool", "tensor_tensor", out=z_[:], in0=z_[:], in1=skb[:], op=ALU.mult, R=[z_, skb], W=[z_])
            P.op("dve", "tensor_tensor", out=o_[:], in0=py[:], in1=z_[:], op=ALU.add, R=[py, z_], W=[o_])
            P.op("dve", "tensor_tensor", out=o_[:], in0=o_[:], in1=g_[:], op=ALU.mult, R=[o_, g_], W=[o_])
            P.dma("sp", dst[nt * 128:(nt + 1) * 128, :], o_[:], R=[o_], W=[P.key("st")])


def stage_hyena(P, D, l, with_ctx):
    tiles = list(range(NT)) if with_ctx else list(range(2, NT))
    stage_hy_conv(P, D, l, tiles)
    seqs = ([(T_CTX, 0)] if with_ctx else []) + [(T_LAT, T_CTX)]
    for (L, row0) in seqs:
        stage_hy_filt(P, D, l, L)
        stage_hy_filt_fft(P, D, L)
        for o in range(2):
            stage_hy_lconv(P, D, l, L, o, row0)
SPEC = {
    "x": ((T_LAT, DM), F32, "ExternalInput"), "ctx": ((T_CTX, DM), F32, "ExternalInput"),
    "c": ((DM,), F32, "ExternalInput"), "c_ctx": ((DM,), F32, "ExternalInput"),
    "ada_w": ((DEPTH, DM, 6 * DM), F32, "ExternalInput"), "ada_b": ((DEPTH, 6 * DM), F32, "ExternalInput"),
    "norm1_g": ((DEPTH, DM), F32, "ExternalInput"), "norm2_g": ((DEPTH, DM), F32, "ExternalInput"),
    "w_in": ((DEPTH, DM, DPROJ), F32, "ExternalInput"),
    "hy_conv_w": ((DEPTH, 3, 1536), F32, "ExternalInput"), "hy_conv_b": ((DEPTH, 1536), F32, "ExternalInput"),
    "hy_f_w1": ((DEPTH, 33, 64), F32, "ExternalInput"), "hy_f_b1": ((DEPTH, 64), F32, "ExternalInput"),
    "hy_f_w2": ((DEPTH, 64, 64), F32, "ExternalInput"), "hy_f_b2": ((DEPTH, 64), F32, "ExternalInput"),
    "hy_f_freq": ((DEPTH, 2, 64), F32, "ExternalInput"), "hy_f_w3": ((DEPTH, 64, 2048), F32, "ExternalInput"),
    "hy_decay": ((DEPTH, 2048), F32, "ExternalInput"), "hy_skip": ((DEPTH, 2, 512), F32, "ExternalInput"),
    "gla_wa2": ((DEPTH, 2, 16, 256), F32, "ExternalInput"), "gla_ba": ((DEPTH, 2, 256), F32, "ExternalInput"),
    "gla_norm_g": ((DEPTH, 512), F32, "ExternalInput"), "ml_gate_b": ((DEPTH, 2, 2, 4), F32, "ExternalInput"),
    "ml_norm_g": ((DEPTH, 512), F32, "ExternalInput"), "ret_decay": ((DEPTH, 2, 4), F32, "ExternalInput"),
    "ret_norm_g": ((DEPTH, 512), F32, "ExternalInput"), "w_out": ((DEPTH, DM, DM), F32, "ExternalInput"),
    "moe_wg": ((DEPTH, DM, 4), F32, "ExternalInput"), "moe_bg": ((DEPTH, 4), F32, "ExternalInput"),
    "moe_we": ((DEPTH, DM, 32), F32, "ExternalInput"), "moe_be": ((DEPTH, 32), F32, "ExternalInput"),
    "moe_w1": ((DEPTH, 4, 8, DM, FF), F32, "ExternalInput"), "moe_w3": ((DEPTH, 4, 8, DM, FF), F32, "ExternalInput"),
    "moe_w2": ((DEPTH, 4, 8, FF, DM), F32, "ExternalInput"), "final_g": ((DM,), F32, "ExternalInput"),
    "k_ident": ((128, 128), F32, "ExternalInput"), "k_tri": ((3, 128, 128), F32, "ExternalInput"),
    "k_sel": ((32, 32, 128), F32, "ExternalInput"),
    "k_cos": ((T_LAT, 64), F32, "ExternalInput"), "k_sin": ((T_LAT, 64), F32, "ExternalInput"),
    "res": ((T_ALL, DM), F32, "Internal"), "modd": ((DEPTH, 2, 6 * DM), F32, "Internal"),
    "p": ((T_ALL, DPROJ), F32, "Internal"), "ymix": ((T_ALL, DM), F32, "Internal"),
    "xn2T": ((128, KC, T_ALL), BF16, "Internal"), "combT": ((32, T_ALL), F32, "Internal"),
    "oacc": ((T_ALL, 512), F32, "Internal"),
    "out": ((T_HALF, DM), F32, "ExternalOutput"),
    "res_h": ((T_HALF, DM), F32, "Internal"),
    "k_rows": ((128, T_HALF // 128), I32, "ExternalInput"),
}


SPEC.update(HY_SPEC)


class DramTable(dict):
    def __init__(self, nc, ext=None):
        super().__init__()
        self.nc, self.ext = nc, dict(ext or {})

    def __missing__(self, name):
        shape, dt, kind = SPEC[name]
        kind = self.ext.get(name, kind)
        ap = self.nc.dram_tensor(name, list(shape), dt, kind=kind).ap()
        self[name] = ap
        return ap


def host_consts():
    j = np.arange(128)[:, None]
    i = np.arange(128)[None, :]
    tri = np.stack([(j <= i), (j >= i), np.ones((128, 128), bool)]).astype(np.float32)
    sel = np.zeros((32, 32, 128), np.float32)
    for e in range(32):
        sel[e, e, :] = 1.0
    tt = np.arange(T_LAT)
    inv = (10000.0 ** (-np.arange(32, dtype=np.float32) / np.float32(32))).astype(np.float32)
    ang = np.concatenate([(tt // 64).astype(np.float32)[:, None] * inv, (tt % 64).astype(np.float32)[:, None] * inv], -1)
    out = {"k_ident": np.eye(128, dtype=np.float32), "k_tri": tri, "k_sel": sel,
           "k_cos": np.cos(ang).astype(np.float32), "k_sin": np.sin(ang).astype(np.float32)}
    out.update(hy_tables(T_LAT))
    out.update(hy_tables(T_CTX))
    return out


def half_rows(h):
    r = T_CTX + h * T_HALF + np.arange(T_HALF, dtype=np.int32)
    return np.ascontiguousarray(r.reshape(T_HALF // 128, 128).T)


def stage_consts(P, D):
    C = {}
    C["idf"] = P.sb([128, 128], F32, name="c_idf")
    C["idb"] = P.sb([128, 128], BF16, name="c_idb")
    with P.stage("consts"):
        P.dma("sp", C["idf"][:], D["k_ident"], W=[C["idf"]])
        P.op("dve", "tensor_copy", out=C["idb"][:], in_=C["idf"][:], R=[C["idf"]], W=[C["idb"]])
    return C


def build_program():
    nc = bass.Bass("TRN2", target_bir_lowering=False)
    P = Prog(nc)
    D = DramTable(nc)
    C = stage_consts(P, D)
    stage_init(P, D)
    lat_tiles = list(range(2, NT))
    lat_groups = [lat_tiles[i:i + 8] for i in range(0, len(lat_tiles), 8)]
    for l in range(DEPTH):
        with_ctx = l < DEPTH - 1
        stage_mod(P, D, l)
        stage_inproj(P, D, l, C)
        stage_hyena(P, D, l, with_ctx)
        for kind in ("gla", "ml", "ret"):
            for dr in range(2):
                stage_linattn(P, D, l, C, kind, dr, with_ctx)
        if with_ctx:
            tiles = list(range(NT))
            stage_outproj(P, D, l, C, tiles)
            stage_moe_a(P, D, l, C, tiles)
            stage_moe_b(P, D, l, C, [tiles[i:i + 7] for i in range(0, NT, 7)])
        else:
            ht = list(range(T_HALF // 128))
            stage_outproj(P, D, l, C, ht, gather=True)
            stage_moe_a(P, D, l, C, ht, res="res_h", lat_only=True)
            stage_moe_b(P, D, l, C, [ht[i:i + 8] for i in range(0, len(ht), 8)], res="res_h", lat_only=True)
    stage_final(P, D)
    P.finish()
    P.close()
    in_names = [n for n in D if SPEC[n][2] == "ExternalInput"]
    return nc, in_names


_CACHE = {}


def kernel(**inputs):
    if "prog" not in _CACHE:
        _CACHE["prog"] = build_program()
        _CACHE["consts"] = host_consts()
    nc, in_names = _CACHE["prog"]
    consts = _CACHE["consts"]
    n_cores = 8
    shared = {}
    for n in in_names:
        if n in consts:
            shared[n] = consts[n]
        elif n not in ("x", "ctx", "c", "k_rows"):
            shared[n] = np.ascontiguousarray(np.asarray(inputs[n], dtype=np.float32))
    in_maps = []
    for i in range(n_cores):
        b = i // 2
        m = dict(shared)
        for n in ("x", "ctx", "c"):
            m[n] = np.ascontiguousarray(np.asarray(inputs[n][b], dtype=np.float32))
        m["k_rows"] = half_rows(i % 2)
        in_maps.append(m)
    res = run_bass_kernel_spmd(nc, in_maps, core_ids=list(range(n_cores)))
    out = np.stack([np.concatenate([np.asarray(res.results[2 * b + h]["out"]) for h in range(2)], axis=0)
                    for b in range(4)], axis=0)
    return out.astype(np.float32)
```
